# Optimizing a Trainium2 kernel written in Bass

```python
import math
import jax, jax.numpy as jnp
from jax import lax
import numpy as np

D_MODEL = 1024
BATCH = 4
SEQ = 4096
DEPTH = 4

ROPE_THETA = 10000.0
LN_EPS = 1e-5
ATT_GROUPS = ((128, 1), (512, 4), (2048, 16))
ATT_HEADS_PER_GROUP = 4
ATT_HEADS = ATT_HEADS_PER_GROUP * len(ATT_GROUPS)
ATT_HEAD_DIM = 128
ATT_BLOCK = 128
ATT_OUT = ATT_HEADS_PER_GROUP * ATT_HEAD_DIM
LRU_WIDTH = D_MODEL
LRU_BLOCKS = 16
LRU_BLOCK_DIM = LRU_WIDTH // LRU_BLOCKS
CONV_WIDTH = 4
LRU_C = 8.0
RET_HEADS = 4
RET_DK = 256
RET_DV = 256
RET_CHUNK = 128
PEER_HEADS = 8
PEER_NKEYS = 128
PEER_EXPERTS = PEER_NKEYS * PEER_NKEYS
PEER_QDIM = 256
PEER_TOPK = 16
PEER_BLOCK = 128
DEEPNORM_ALPHA = (2 * DEPTH) ** 0.25
DEEPNORM_BETA = (8 * DEPTH) ** -0.25
IN_SPLITS = (ATT_HEADS * ATT_HEAD_DIM,) * 3 + (LRU_WIDTH,) * 2 + (RET_HEADS * RET_DK, RET_HEADS * RET_DK, RET_HEADS * RET_DV, RET_HEADS * RET_DV) + (D_MODEL,) * 3
D_IN = sum(IN_SPLITS)
BR_SPLITS = (ATT_OUT, LRU_WIDTH, RET_HEADS * RET_DV)
D_BR = sum(BR_SPLITS)

kernel_name = "hybrid_dilated_lru_retention_peer"


def layer_norm(x, g, b):
    xf = x.astype(jnp.float32)
    mu = jnp.mean(xf, axis=-1, keepdims=True)
    var = jnp.mean(jnp.square(xf - mu), axis=-1, keepdims=True)
    return ((xf - mu) * lax.rsqrt(var + LN_EPS) * g + b).astype(x.dtype)


def rope_tables(seq, dim):
    inv = ROPE_THETA ** (-jnp.arange(0, dim, 2, dtype=jnp.float32) / dim)
    ang = jnp.arange(seq, dtype=jnp.float32)[:, None] * inv[None, :]
    return jnp.cos(ang), jnp.sin(ang)


def apply_rope(t, cos, sin):
    half = t.shape[-1] // 2
    t1 = t[..., :half].astype(jnp.float32)
    t2 = t[..., half:].astype(jnp.float32)
    c = cos[None, :, None, :]
    s = sin[None, :, None, :]
    return jnp.concatenate([t1 * c - t2 * s, t1 * s + t2 * c], axis=-1).astype(t.dtype)


def dilated_group(q, k, v, window, dil):
    B, S, H, E = q.shape
    span = dil * ATT_BLOCK
    Sp = -(-S // span) * span
    L = Sp // dil
    nb = L // ATT_BLOCK
    sub_win = window // dil

    def prep(t):
        t = jnp.pad(t, ((0, 0), (0, Sp - S), (0, 0), (0, 0)))
        t = t.reshape(B, L, dil, H, E).transpose(0, 2, 3, 1, 4)
        return t.reshape(B, dil, H, nb, ATT_BLOCK, E)

    qb, kb, vb = prep(q), prep(k), prep(v)
    pad_prev = ((0, 0), (0, 0), (0, 0), (1, 0), (0, 0), (0, 0))
    kcat = jnp.concatenate([jnp.pad(kb[:, :, :, :-1], pad_prev), kb], axis=4)
    vcat = jnp.concatenate([jnp.pad(vb[:, :, :, :-1], pad_prev), vb], axis=4)
    s = jnp.einsum('bdhnqe,bdhnke->bdhnqk', qb, kcat).astype(jnp.float32) * (E ** -0.5)
    qi = jnp.arange(ATT_BLOCK)[:, None] + ATT_BLOCK
    kj = jnp.arange(2 * ATT_BLOCK)[None, :]
    delta = qi - kj
    blk = jnp.arange(nb)[:, None, None]
    mask = (delta >= 0)[None] & (delta <= sub_win)[None] & (blk * ATT_BLOCK + kj[None] - ATT_BLOCK >= 0)
    s = jnp.where(mask, s, -jnp.inf)
    lse = jax.nn.logsumexp(s, axis=-1, keepdims=True)
    p = jnp.exp(s - lse)
    o = jnp.einsum('bdhnqk,bdhnke->bdhnqe', p.astype(vcat.dtype), vcat)
    o = o.reshape(B, dil, H, L, E).transpose(0, 3, 1, 2, 4).reshape(B, Sp, H, E)[:, :S]
    lse = lse[..., 0].reshape(B, dil, H, L).transpose(0, 3, 1, 2).reshape(B, Sp, H)[:, :S]
    return o, lse


def rg_lru(xc, wa, ba, wx, bx, lam):
    B, S, C = xc.shape
    xr = xc.reshape(B, S, LRU_BLOCKS, LRU_BLOCK_DIM)
    r = jax.nn.sigmoid((jnp.einsum('bsgi,gio->bsgo', xr, wa).reshape(B, S, C) + ba).astype(jnp.float32))
    i = jax.nn.sigmoid((jnp.einsum('bsgi,gio->bsgo', xr, wx).reshape(B, S, C) + bx).astype(jnp.float32))
    log_a = -LRU_C * r * jax.nn.softplus(-lam.astype(jnp.float32))
    a = jnp.exp(log_a)
    b = jnp.sqrt(-jnp.expm1(2.0 * log_a)) * (i * xc.astype(jnp.float32))

    def combine(left, right):
        a1, b1 = left
        a2, b2 = right
        return a1 * a2, a2 * b1 + b2

    _, h = lax.associative_scan(combine, (a, b), axis=1)
    return h


def retention(q, k, v):
    B, S, H, Dk = q.shape
    Dv = v.shape[-1]
    n = S // RET_CHUNK
    log_g = jnp.log(1.0 - 2.0 ** (-5.0 - jnp.arange(H, dtype=jnp.float32)))
    idx = jnp.arange(RET_CHUNK, dtype=jnp.float32)
    diff = idx[:, None] - idx[None, :]
    dmat = jnp.where(diff >= 0, jnp.exp(jnp.maximum(diff, 0.0)[None] * log_g[:, None, None]), 0.0)
    xi = jnp.exp((idx[None] + 1.0) * log_g[:, None])[..., None]
    zeta = jnp.exp((RET_CHUNK - 1.0 - idx[None]) * log_g[:, None])[..., None]
    g_chunk = jnp.exp(RET_CHUNK * log_g)[:, None, None]

    def chunks(t):
        return t.reshape(B, n, RET_CHUNK, H, t.shape[-1]).transpose(1, 0, 3, 2, 4)

    def step(R, inp):
        qc, kc, vc = inp
        inner = jnp.einsum('bhij,bhjv->bhiv', jnp.einsum('bhik,bhjk->bhij', qc, kc) * dmat, vc)
        cross = jnp.einsum('bhik,bhkv->bhiv', qc, R) * xi
        R_new = g_chunk * R + jnp.einsum('bhjk,bhjv->bhkv', kc * zeta, vc)
        return R_new, inner + cross

    R0 = jnp.zeros((B, H, Dk, Dv), jnp.float32)
    _, out = lax.scan(step, R0, (chunks(q), chunks(k), chunks(v)))
    return out.transpose(1, 0, 3, 2, 4).reshape(B, S, H, Dv)


def token_mixers(x, w_in, conv_w, conv_b, lru_wa, lru_ba, lru_wx, lru_bx, lru_lambda, w_branch, w_out, cos_a, sin_a, cos_r, sin_r):
    B, S, _ = x.shape
    proj = jnp.einsum('bsd,de->bse', x, w_in)
    aq, ak, av, lx, lg, rq, rk, rv, rg, ga, gb, gc = jnp.split(proj, np.cumsum(IN_SPLITS)[:-1].tolist(), axis=-1)

    aq = apply_rope(aq.reshape(B, S, ATT_HEADS, ATT_HEAD_DIM), cos_a, sin_a)
    ak = apply_rope(ak.reshape(B, S, ATT_HEADS, ATT_HEAD_DIM), cos_a, sin_a)
    av = av.reshape(B, S, ATT_HEADS, ATT_HEAD_DIM)
    outs, lses = [], []
    for g, (win, dil) in enumerate(ATT_GROUPS):
        sl = slice(g * ATT_HEADS_PER_GROUP, (g + 1) * ATT_HEADS_PER_GROUP)
        o, l = dilated_group(aq[:, :, sl], ak[:, :, sl], av[:, :, sl], win, dil)
        outs.append(o)
        lses.append(l)
    wgt = jax.nn.softmax(jnp.stack(lses), axis=0)
    y_a = jnp.sum(wgt[..., None] * jnp.stack(outs).astype(jnp.float32), axis=0)
    y_a = y_a.reshape(B, S, ATT_OUT).astype(x.dtype)

    xc = lax.conv_general_dilated(lx, conv_w[:, None, :], window_strides=(1,), padding=[(CONV_WIDTH - 1, 0)],
                                  dimension_numbers=('NWC', 'WIO', 'NWC'), feature_group_count=LRU_WIDTH) + conv_b
    h = rg_lru(xc, lru_wa, lru_ba, lru_wx, lru_bx, lru_lambda)
    y_b = (h * jax.nn.gelu(lg.astype(jnp.float32), approximate=False)).astype(x.dtype)

    rq = apply_rope(rq.reshape(B, S, RET_HEADS, RET_DK), cos_r, sin_r).astype(jnp.float32)
    rk = apply_rope(rk.reshape(B, S, RET_HEADS, RET_DK), cos_r, sin_r).astype(jnp.float32) * (RET_DK ** -0.5)
    rv = rv.reshape(B, S, RET_HEADS, RET_DV).astype(jnp.float32)
    ret = retention(rq, rk, rv)
    mu = jnp.mean(ret, axis=-1, keepdims=True)
    var = jnp.mean(jnp.square(ret - mu), axis=-1, keepdims=True)
    ret = ((ret - mu) * lax.rsqrt(var + LN_EPS)).reshape(B, S, RET_HEADS * RET_DV)
    y_c = (jax.nn.silu(rg.astype(jnp.float32)) * ret).astype(x.dtype)

    wb_a, wb_b, wb_c = jnp.split(w_branch, np.cumsum(BR_SPLITS)[:-1].tolist(), axis=0)
    merged = (jax.nn.sigmoid(ga) * (y_a @ wb_a) + jax.nn.sigmoid(gb) * (y_b @ wb_b)
              + jax.nn.sigmoid(gc) * (y_c @ wb_c))
    return merged @ w_out


def peer_ffn(x, wq, k1, k2, U, V):
    B, S, D = x.shape
    xt = x.reshape(-1, D)
    N = xt.shape[0]
    q = (xt @ wq).astype(jnp.float32).reshape(N, PEER_HEADS, PEER_QDIM)
    half = PEER_QDIM // 2
    s1 = jnp.einsum('nhc,hkc->nhk', q[..., :half], k1.astype(jnp.float32))
    s2 = jnp.einsum('nhc,hkc->nhk', q[..., half:], k2.astype(jnp.float32))
    v1, i1 = lax.top_k(s1, PEER_TOPK)
    v2, i2 = lax.top_k(s2, PEER_TOPK)
    cand = (v1[..., :, None] + v2[..., None, :]).reshape(N, PEER_HEADS, PEER_TOPK * PEER_TOPK)
    cv, ci = lax.top_k(cand, PEER_TOPK)
    e = (jnp.take_along_axis(i1, ci // PEER_TOPK, axis=-1) * PEER_NKEYS
         + jnp.take_along_axis(i2, ci % PEER_TOPK, axis=-1))
    g = jax.nn.softmax(cv, axis=-1)
    nb = N // PEER_BLOCK

    def block(args):
        xb, eb, gbk = args
        u = U[eb]
        pre = jnp.einsum('thkd,td->thk', u, xb).astype(jnp.float32)
        act = (jax.nn.gelu(pre, approximate=False) * gbk).astype(V.dtype)
        return jnp.einsum('thk,thkd->td', act, V[eb])

    y = lax.map(block, (xt.reshape(nb, PEER_BLOCK, D), e.reshape(nb, PEER_BLOCK, PEER_HEADS, PEER_TOPK),
                        g.reshape(nb, PEER_BLOCK, PEER_HEADS, PEER_TOPK)))
    return y.reshape(B, S, D).astype(x.dtype)


def setup_inputs(seed: int = 0) -> dict:
    key = jax.random.key(seed)
    ks = jax.random.split(key, 24)
    D = D_MODEL
    nrm = jax.random.normal
    x = nrm(ks[0], (BATCH, SEQ, D), jnp.float32)
    w_in = nrm(ks[1], (DEPTH, D, D_IN), jnp.float32) * D ** -0.5
    conv_w = nrm(ks[2], (DEPTH, CONV_WIDTH, LRU_WIDTH), jnp.float32) * CONV_WIDTH ** -0.5
    conv_b = 0.01 * nrm(ks[3], (DEPTH, LRU_WIDTH), jnp.float32)
    lru_wa = nrm(ks[4], (DEPTH, LRU_BLOCKS, LRU_BLOCK_DIM, LRU_BLOCK_DIM), jnp.float32) * LRU_BLOCK_DIM ** -0.5
    lru_ba = 0.01 * nrm(ks[5], (DEPTH, LRU_WIDTH), jnp.float32)
    lru_wx = nrm(ks[6], (DEPTH, LRU_BLOCKS, LRU_BLOCK_DIM, LRU_BLOCK_DIM), jnp.float32) * LRU_BLOCK_DIM ** -0.5
    lru_bx = 0.01 * nrm(ks[7], (DEPTH, LRU_WIDTH), jnp.float32)
    a_c = jax.random.uniform(ks[8], (DEPTH, LRU_WIDTH), jnp.float32, minval=0.9, maxval=0.999)
    a0 = a_c ** (1.0 / LRU_C)
    lru_lambda = jnp.log(a0) - jnp.log1p(-a0)
    w_branch = jnp.concatenate([
        nrm(ks[9], (DEPTH, BR_SPLITS[0], D), jnp.float32) * BR_SPLITS[0] ** -0.5,
        nrm(ks[10], (DEPTH, BR_SPLITS[1], D), jnp.float32) * BR_SPLITS[1] ** -0.5,
        nrm(ks[11], (DEPTH, BR_SPLITS[2], D), jnp.float32) * BR_SPLITS[2] ** -0.5], axis=1)
    w_out = nrm(ks[12], (DEPTH, D, D), jnp.float32) * D ** -0.5 * DEEPNORM_BETA
    ln1_g = 1.0 + 0.02 * nrm(ks[13], (DEPTH, D), jnp.float32)
    ln1_b = 0.02 * nrm(ks[14], (DEPTH, D), jnp.float32)
    peer_wq = nrm(ks[15], (DEPTH, D, PEER_HEADS * PEER_QDIM), jnp.float32) * D ** -0.5
    peer_k1 = nrm(ks[16], (DEPTH, PEER_HEADS, PEER_NKEYS, PEER_QDIM // 2), jnp.float32) * (PEER_QDIM // 2) ** -0.5
    peer_k2 = nrm(ks[17], (DEPTH, PEER_HEADS, PEER_NKEYS, PEER_QDIM // 2), jnp.float32) * (PEER_QDIM // 2) ** -0.5
    peer_u = nrm(ks[18], (DEPTH, PEER_EXPERTS, D), jnp.float32) * D ** -0.5
    peer_v = nrm(ks[19], (DEPTH, PEER_EXPERTS, D), jnp.float32) * PEER_HEADS ** -0.5 * DEEPNORM_BETA
    ln2_g = 1.0 + 0.02 * nrm(ks[20], (DEPTH, D), jnp.float32)
    ln2_b = 0.02 * nrm(ks[21], (DEPTH, D), jnp.float32)
    return {"x": x, "w_in": w_in, "conv_w": conv_w, "conv_b": conv_b, "lru_wa": lru_wa, "lru_ba": lru_ba,
            "lru_wx": lru_wx, "lru_bx": lru_bx, "lru_lambda": lru_lambda, "w_branch": w_branch, "w_out": w_out,
            "ln1_g": ln1_g, "ln1_b": ln1_b, "peer_wq": peer_wq, "peer_k1": peer_k1, "peer_k2": peer_k2,
            "peer_u": peer_u, "peer_v": peer_v, "ln2_g": ln2_g, "ln2_b": ln2_b}


def reference(x, w_in, conv_w, conv_b, lru_wa, lru_ba, lru_wx, lru_bx, lru_lambda, w_branch, w_out,
              ln1_g, ln1_b, peer_wq, peer_k1, peer_k2, peer_u, peer_v, ln2_g, ln2_b):
    S = x.shape[1]
    cos_a, sin_a = rope_tables(S, ATT_HEAD_DIM)
    cos_r, sin_r = rope_tables(S, RET_DK)
    for l in range(DEPTH):
        m = token_mixers(x, w_in[l], conv_w[l], conv_b[l], lru_wa[l], lru_ba[l], lru_wx[l], lru_bx[l],
                         lru_lambda[l], w_branch[l], w_out[l], cos_a, sin_a, cos_r, sin_r)
        x = layer_norm(DEEPNORM_ALPHA * x + m, ln1_g[l], ln1_b[l])
        f = peer_ffn(x, peer_wq[l], peer_k1[l], peer_k2[l], peer_u[l], peer_v[l])
        x = layer_norm(DEEPNORM_ALPHA * x + f, ln2_g[l], ln2_b[l])
    return x
```

```python
from contextlib import ExitStack
import math
import os
import numpy as np
import ml_dtypes
import concourse.bass as bass
import concourse.mybir as mybir
from concourse.bass_utils import run_bass_kernel_spmd

F32 = mybir.dt.float32
BF16 = mybir.dt.bfloat16
I32 = mybir.dt.int32
U32 = mybir.dt.uint32
AF = mybir.ActivationFunctionType
ALU = mybir.AluOpType
AX = mybir.AxisListType

S = 4096
D = 1024
NT = S // 128
DEPTH = 4
ALPHA = (2 * DEPTH) ** 0.25
LN_EPS = 1e-5


class Buf:
    def __init__(self, name, ap):
        self.name = name
        self.ap = ap
        self.w = None
        self.r = {}
        self.dsem = None
        self.dcnt = 0

    def __getitem__(self, idx):
        return self.ap[idx]


class KB:
    CE = ("pe", "dve", "act", "pool")

    def __init__(self, nc):
        self.nc = nc
        self.e = dict(pe=nc.tensor, dve=nc.vector, act=nc.scalar, pool=nc.gpsimd, sp=nc.sync)
        self.sem = {k: nc.alloc_semaphore("c_" + k) for k in self.CE}
        self.bar = nc.alloc_semaphore("bar")
        self.nbar = 0
        self.cnt = {k: 0 for k in self.CE}
        self.seen = {}
        self.bufs = []
        self.uid = 0
        self.dsems = []
        self.sem_pool = []
        self.n_ops = 0

    def _nm(self, name):
        self.uid += 1
        return f"{name}_{self.uid}"

    def sb(self, name, shape, dt, es=None, side=None):
        nm = self._nm(name)
        kw = {} if side is None else {"side": side}
        if es is None:
            t = self.nc.alloc_sbuf_tensor(nm, list(shape), dt, **kw)
        else:
            t = es.enter_context(self.nc.sbuf_tensor(nm, list(shape), dt, **kw))
        b = Buf(nm, t.ap())
        self.bufs.append(b)
        return b

    def ps(self, name, shape, dt=F32, es=None):
        nm = self._nm(name)
        if es is None:
            t = self.nc.alloc_psum_tensor(nm, list(shape), dt)
        else:
            t = es.enter_context(self.nc.psum_tensor(nm, list(shape), dt))
        b = Buf(nm, t.ap())
        self.bufs.append(b)
        return b

    def dram(self, name, shape, dt, kind="Internal"):
        t = self.nc.dram_tensor(name, list(shape), dt, kind=kind)
        b = Buf(name, t.ap())
        self.bufs.append(b)
        return b

    def _wait(self, eng, tok):
        if tok[0] == "c":
            _, src, kk = tok
            if src == eng and eng == "pe":
                return
            key = (eng, "c", src)
            if self.seen.get(key, 0) >= kk:
                return
            self.seen[key] = kk
            self.e[eng].wait_ge(self.sem[src], kk)
        else:
            _, buf = tok
            key = (eng, "d", buf.name)
            kk = buf.dcnt
            if self.seen.get(key, 0) >= kk:
                return
            self.seen[key] = kk
            self.e[eng].wait_ge(buf.dsem, kk)

    def _deps(self, eng, reads, writes, nowaw=False):
        toks = []
        for b in reads:
            if b.w is not None:
                toks.append(b.w)
        for b in writes:
            if b.w is not None and not (nowaw and b.w[0] == "c" and b.w[1] == eng):
                toks.append(b.w)
            toks.extend(b.r.values())
        for t in toks:
            self._wait(eng, t)

    @staticmethod
    def _mark(tok, key, reads, writes):
        for b in reads:
            b.r[key] = tok
        for b in writes:
            b.w = tok
            b.r = {}

    def op(self, eng, fn, reads=(), writes=(), nowaw=False):
        self._deps(eng, reads, writes, nowaw)
        ins = fn(self.e[eng])
        self.cnt[eng] += 1
        ins.then_inc(self.sem[eng], 1)
        self._mark(("c", eng, self.cnt[eng]), eng, reads, writes)
        self.n_ops += 1
        return ins

    def dma(self, q, out_ap, in_ap, sbuf, reads=(), writes=(), indirect=None, **kw):
        self._deps(q, reads, writes)
        if sbuf.dsem is None:
            if self.sem_pool:
                sbuf.dsem, sbuf.dcnt = self.sem_pool.pop()
            else:
                sbuf.dsem = self.nc.alloc_semaphore("d_" + sbuf.name)
            self.dsems.append(sbuf)
        if indirect is None:
            ins = self.e[q].dma_start(out=out_ap, in_=in_ap, **kw)
        else:
            ins = self.e[q].indirect_dma_start(out=out_ap, out_offset=None, in_=in_ap, in_offset=indirect, **kw)
        sbuf.dcnt += 16
        ins.then_inc(sbuf.dsem, 16)
        self._mark(("d", sbuf), "d_" + sbuf.name, reads, writes)
        self.n_ops += 1
        return ins

    def coll(self, kind, op, groups, src, dst):
        self._deps("pool", [src], [dst])
        if dst.dsem is None:
            dst.dsem = self.nc.alloc_semaphore("d_" + dst.name)
            self.dsems.append(dst)
        ins = self.e["pool"].collective_compute(kind, op, replica_groups=groups, ins=[src.ap[:, :]], outs=[dst.ap[:, :]])
        dst.dcnt += 16
        ins.then_inc(dst.dsem, 16)
        self._mark(("d", dst), "d_" + dst.name, [src], [dst])
        return ins

    def barrier(self):
        sp = self.e["sp"]
        for e in self.CE:
            if self.cnt[e] > self.seen.get(("sp", "c", e), 0):
                self.seen[("sp", "c", e)] = self.cnt[e]
                sp.wait_ge(self.sem[e], self.cnt[e])
        for b in self.dsems:
            if b.dcnt > self.seen.get(("sp", "d", b.name), 0):
                self.seen[("sp", "d", b.name)] = b.dcnt
                sp.wait_ge(b.dsem, b.dcnt)
        self.nbar += 1
        sp.sem_inc(self.bar, 1)
        for e in self.CE:
            self.e[e].wait_ge(self.bar, self.nbar)
        for b in self.bufs:
            b.w = None
            b.r = {}

    def mark(self):
        return len(self.bufs)

    def release(self, marker):
        self.barrier()
        gone = self.bufs[marker:]
        del self.bufs[marker:]
        for b in gone:
            if b.dsem is not None:
                self.sem_pool.append((b.dsem, b.dcnt))
                self.dsems.remove(b)

    def view(self, buf, ap):
        return Buf(buf.name + "_v", ap)

    def finish(self):
        self.barrier()
        self.e["sp"].wait_ge(self.bar, self.nbar)


STG_N = 3072


class WLoader:
    def __init__(self, k, es, n=2, side=None):
        self.k = k
        self.stg = [k.sb("stg", [128, STG_N], F32, es, side=side) for _ in range(n)]
        self.i = 0

    def load(self, dst, dst_ap_fn, src_buf, src_rows0, kc, col0, ncols, q="sp"):
        k = self.k
        per = max(1, STG_N // ncols)
        c0 = 0
        while c0 < kc:
            c1 = min(kc, c0 + per)
            st = self.stg[self.i % len(self.stg)]
            self.i += 1
            src = src_buf.ap[src_rows0 + c0 * 128: src_rows0 + c1 * 128, col0:col0 + ncols].rearrange(
                "(c p) n -> p c n", p=128)
            sv = st.ap[:, 0:(c1 - c0) * ncols].rearrange("p (c n) -> p c n", n=ncols)
            k.dma(q, sv, src, st, reads=[src_buf], writes=[st])
            dv = dst_ap_fn(c0, c1)
            k.op("pool", lambda e: e.tensor_copy(out=dv, in_=sv), reads=[st], writes=[dst])
            c0 = c1


def mixer_body(k, T, gates="compute", bg=None):
    x, w_att, w_lru, w_ret, w_gate, w_br, w_out = (T[n] for n in ("x", "w_att", "w_lru", "w_ret", "w_gate", "w_br", "w_out"))
    lru_vec, wa_bd, wx_bd, tabA, tabR, dec, cst = (T[n] for n in ("lru_vec", "wa_bd", "wx_bd", "tabA", "tabR", "dec", "cst"))
    yaT, ybT, ycT, m_part = (T[n] for n in ("yaT", "ybT", "ycT", "m_part"))
    sgT = T["sgT"]
    mk0 = k.mark()
    eso = ExitStack()

    xT = k.sb("xT", [128, 8, S], BF16, eso)
    cs = k.sb("cs", [128, 512], F32, eso)
    identb = k.sb("identb", [128, 128], BF16, eso)
    onesb = k.sb("onesb", [128, 128], BF16, eso)
    decs = k.sb("decs", [128, 6], F32, eso)
    k.dma("sp", cs[:], cst[:], cs, reads=[cst], writes=[cs])
    k.dma("sp", decs[:], dec[:], decs, reads=[dec], writes=[decs])
    k.op("pool", lambda e: e.tensor_copy(out=identb[:], in_=cs[:, 0:128]), reads=[cs], writes=[identb])
    k.op("pool", lambda e: e.memset(onesb[:], 1.0), writes=[onesb])
    ident = cs

    with ExitStack() as es:
        xs = [k.sb("xs", [128, D], F32, es) for _ in range(2)]
        pT = [k.ps("pT", [128, 8, 128], F32, es) for _ in range(2)]
        for t in range(NT):
            xb = xs[t % 2]
            pb = pT[t % 2]
            k.dma("sp", xb[:], x[t * 128:(t + 1) * 128, :], xb, reads=[x], writes=[xb])
            for c in range(8):
                k.op("pe", lambda e: e.transpose(pb[:, c, :], xb[:, c * 128:(c + 1) * 128], ident[:, 0:128]),
                     reads=[xb, cs], writes=[pb])
            eng = "dve" if t % 2 == 0 else "act"
            if eng == "dve":
                k.op("dve", lambda e: e.tensor_copy(out=xT[:, :, t * 128:(t + 1) * 128], in_=pb[:]),
                     reads=[pb], writes=[xT])
            else:
                k.op("act", lambda e: e.activation(out=xT[:, :, t * 128:(t + 1) * 128], in_=pb[:], func=AF.Copy),
                     reads=[pb], writes=[xT])
        k.barrier()

    if gates == "compute":
        with ExitStack() as es:
            wl = WLoader(k, es)
            wgt = k.sb("wgt", [128, 8, 3072], BF16, es)
            for c0 in range(8):
                wl.load(wgt, lambda a, b, c0=c0: wgt[:, c0 + a:c0 + b, :], w_gate, c0 * 128, 1, 0, 3072)
            sgs = [k.sb("sgs", [128, 512], BF16, es) for _ in range(4)]
            pg_ = [k.ps("pgG", [128, 512], F32, es) for _ in range(4)]
            gi_ = 0
            for tg in range(8):
                tok = slice(tg * 512, (tg + 1) * 512)
                for m3 in range(24):
                    pb = pg_[gi_ % 4]
                    sb_ = sgs[gi_ % 4]
                    gi_ += 1
                    for c in range(8):
                        k.op("pe", lambda e: e.matmul(pb[:], lhsT=wgt[:, c, m3 * 128:(m3 + 1) * 128], rhs=xT[:, c, tok],
                                                      start=(c == 0), stop=(c == 7)), reads=[wgt, xT], writes=[pb])
                    k.op("act", lambda e: e.activation(out=sb_[:], in_=pb[:], func=AF.Sigmoid), reads=[pb], writes=[sb_])
                    k.dma("sp", sgT[m3 * 128:(m3 + 1) * 128, tok], sb_[:], sb_, reads=[sb_], writes=[sgT])
            k.barrier()

    if os.environ.get('MIX_STOP') == '0':
        eso.close()
        k.release(mk0)
        return
    TL = 1024
    with ExitStack() as es:
        wl = WLoader(k, es)
        wlru = k.sb("wlru", [128, 8, 1024], BF16, es)
        wl.load(wlru, lambda c0, c1: wlru[:, c0:c1, :], w_lru, 0, 8, 0, 1024)
        wabd = k.sb("wabd", [128, 4, 128], BF16, es)
        wxbd = k.sb("wxbd", [128, 4, 128], BF16, es)
        wl.load(wabd, lambda c0, c1: wabd[:, c0:c1, :], wa_bd, 0, 4, 0, 128)
        wl.load(wxbd, lambda c0, c1: wxbd[:, c0:c1, :], wx_bd, 0, 4, 0, 128)
        lv = k.sb("lv", [128, 4, 8], F32, es)
        k.dma("sp", lv[:], lru_vec.ap.rearrange("(c p) k -> p c k", p=128), lv, reads=[lru_vec], writes=[lv])
        ev = k.sb("ev", [128, 4], F32, es)
        zv = k.sb("zv", [128, 4], F32, es)
        z2 = k.sb("z2", [128, 4], F32, es)
        pv_ = k.sb("pv_", [128, 4], F32, es)
        nsp8 = k.sb("nsp8", [128, 4], F32, es)
        k.op("act", lambda e: e.activation(out=ev[:], in_=lv[:, :, 7], func=AF.Exp, scale=-1.0), reads=[lv], writes=[ev])
        k.op("dve", lambda e: e.tensor_scalar(out=zv[:], in0=ev[:], scalar1=2.0, scalar2=None, op0=ALU.add),
             reads=[ev], writes=[zv])
        k.op("dve", lambda e: e.reciprocal(out=zv[:], in_=zv[:]), reads=[zv], writes=[zv])
        k.op("dve", lambda e: e.tensor_tensor(out=zv[:], in0=zv[:], in1=ev[:], op=ALU.mult), reads=[zv, ev], writes=[zv])
        k.op("dve", lambda e: e.tensor_tensor(out=z2[:], in0=zv[:], in1=zv[:], op=ALU.mult), reads=[zv], writes=[z2])
        k.op("dve", lambda e: e.tensor_scalar(out=pv_[:], in0=z2[:], scalar1=1.0 / 7, scalar2=1.0 / 5, op0=ALU.mult, op1=ALU.add),
             reads=[z2], writes=[pv_])
        k.op("dve", lambda e: e.tensor_tensor(out=pv_[:], in0=pv_[:], in1=z2[:], op=ALU.mult), reads=[pv_, z2], writes=[pv_])
        k.op("dve", lambda e: e.tensor_scalar(out=pv_[:], in0=pv_[:], scalar1=1.0 / 3, scalar2=None, op0=ALU.add),
             reads=[pv_], writes=[pv_])
        k.op("dve", lambda e: e.tensor_tensor(out=pv_[:], in0=pv_[:], in1=z2[:], op=ALU.mult), reads=[pv_, z2], writes=[pv_])
        k.op("dve", lambda e: e.tensor_scalar(out=pv_[:], in0=pv_[:], scalar1=1.0, scalar2=None, op0=ALU.add),
             reads=[pv_], writes=[pv_])
        k.op("dve", lambda e: e.tensor_tensor(out=pv_[:], in0=pv_[:], in1=zv[:], op=ALU.mult), reads=[pv_, zv], writes=[pv_])
        k.op("dve", lambda e: e.tensor_scalar(out=nsp8[:], in0=pv_[:], scalar1=-16.0, scalar2=None, op0=ALU.mult),
             reads=[pv_], writes=[nsp8])

        lxs = [k.sb("lx", [128, TL + 3], F32, es) for _ in range(2)]
        glus = [k.sb("glu", [128, TL], F32, es) for _ in range(2)]
        xc = k.sb("xc", [128, TL], F32, es)
        xcb = k.sb("xcb", [128, TL], BF16, es)
        rr = k.sb("rr", [128, TL], F32, es)
        ii = k.sb("ii", [128, TL], F32, es)
        tt = k.sb("tt", [128, TL], F32, es)
        ybs = [k.sb("ybs", [128, TL], BF16, es) for _ in range(2)]
        hl = k.sb("hl", [128, 1], F32, es)
        pp = [k.ps("pl", [128, 512], F32, es) for _ in range(4)]
        pgt = [k.ps("plg", [128, 512], F32, es) for _ in range(2)]
        its = [(ch, tg) for ch in range(4) for tg in range(S // TL)]
        cnt = {"pp": 0, "pg": 0}

        def lru_f(i):
            ch, tg = its[i]
            t0 = tg * TL
            lx, glu = lxs[i % 2], glus[i % 2]
            if tg == 0:
                k.op("pool", lambda e: e.memset(lx[:, 0:3], 0.0), writes=[lx])
            else:
                lp = lxs[(i - 1) % 2]
                k.op("pool", lambda e: e.tensor_copy(out=lx[:, 0:3], in_=lp[:, TL:TL + 3]), reads=[lp], writes=[lx])
            for hf in range(TL // 512):
                for which in range(2):
                    pb = pp[cnt["pp"] % 4]
                    cnt["pp"] += 1
                    for c in range(8):
                        k.op("pe", lambda e: e.matmul(pb[:], lhsT=wlru[:, c, which * 512 + ch * 128: which * 512 + (ch + 1) * 128],
                                                      rhs=xT[:, c, t0 + hf * 512: t0 + (hf + 1) * 512],
                                                      start=(c == 0), stop=(c == 7)),
                             reads=[wlru, xT], writes=[pb])
                    if which == 0:
                        k.op("act", lambda e: e.activation(out=lx[:, 3 + hf * 512: 3 + (hf + 1) * 512], in_=pb[:], func=AF.Copy),
                             reads=[pb], writes=[lx])
                    else:
                        k.op("act", lambda e: e.activation(out=glu[:, hf * 512:(hf + 1) * 512], in_=pb[:], func=AF.Gelu),
                             reads=[pb], writes=[glu])

        def lru_g(i):
            ch, tg = its[i]
            t0 = tg * TL
            lx, glu, yb_ = lxs[i % 2], glus[i % 2], ybs[i % 2]
            k.op("dve", lambda e: e.tensor_scalar(out=xc[:], in0=lx[:, 3:3 + TL], scalar1=lv[:, ch, 3:4], scalar2=lv[:, ch, 4:5],
                                                  op0=ALU.mult, op1=ALU.add), reads=[lx, lv], writes=[xc])
            for j in range(3):
                k.op("dve", lambda e: e.scalar_tensor_tensor(out=xc[:], in0=lx[:, j:j + TL], scalar=lv[:, ch, j:j + 1], in1=xc[:],
                                                             op0=ALU.mult, op1=ALU.add), reads=[lx, lv, xc], writes=[xc])
            k.op("pool", lambda e: e.tensor_copy(out=xcb[:], in_=xc[:]), reads=[xc], writes=[xcb])
            for hf in range(TL // 512):
                for which in range(2):
                    pb = pgt[cnt["pg"] % 2]
                    cnt["pg"] += 1
                    wsel = wabd if which == 0 else wxbd
                    k.op("pe", lambda e: e.matmul(pb[:], lhsT=wsel[:, ch, :], rhs=xcb[:, hf * 512:(hf + 1) * 512], start=True, stop=True),
                         reads=[wsel, xcb], writes=[pb])
                    dst = rr if which == 0 else ii
                    bcol = 5 if which == 0 else 6
                    k.op("act", lambda e: e.activation(out=dst[:, hf * 512:(hf + 1) * 512], in_=pb[:], func=AF.Sigmoid,
                                                       bias=lv[:, ch, bcol:bcol + 1], scale=1.0), reads=[pb, lv], writes=[dst])
            k.op("act", lambda e: e.activation(out=rr[:], in_=rr[:], func=AF.Exp, scale=nsp8[:, ch:ch + 1]),
                 reads=[rr, nsp8], writes=[rr])
            k.op("dve", lambda e: e.tensor_tensor(out=tt[:], in0=rr[:], in1=rr[:], op=ALU.mult), reads=[rr], writes=[tt])
            k.op("act", lambda e: e.activation(out=tt[:], in_=tt[:], func=AF.Sqrt, scale=-1.0, bias=1.0), reads=[tt], writes=[tt])
            k.op("pool", lambda e: e.tensor_tensor(out=ii[:], in0=ii[:], in1=xc[:], op=ALU.mult), reads=[ii, xc], writes=[ii])
            k.op("dve", lambda e: e.tensor_tensor(out=tt[:], in0=tt[:], in1=ii[:], op=ALU.mult), reads=[tt, ii], writes=[tt])
            init = 0.0 if tg == 0 else hl[:, 0:1]
            k.op("dve", lambda e: e.tensor_tensor_scan(out=xc[:], data0=rr[:], data1=tt[:], initial=init, op0=ALU.mult, op1=ALU.add),
                 reads=[rr, tt, hl], writes=[xc])
            k.op("dve", lambda e: e.tensor_copy(out=hl[:], in_=xc[:, TL - 1:TL]), reads=[xc], writes=[hl])
            k.op("dve", lambda e: e.tensor_tensor(out=yb_[:], in0=xc[:], in1=glu[:], op=ALU.mult), reads=[xc, glu], writes=[yb_])
            k.dma("pool", ybT[ch * 128:(ch + 1) * 128, t0:t0 + TL], yb_[:], yb_, reads=[yb_], writes=[ybT])

        bgl = bg(es) if bg is not None else None
        lru_f(0)
        for i in range(len(its)):
            if i + 1 < len(its):
                lru_f(i + 1)
            for _ in range(4):
                if bgl is not None and next(bgl, "END") == "END":
                    bgl = None
            lru_g(i)
            for _ in range(4):
                if bgl is not None and next(bgl, "END") == "END":
                    bgl = None
        if bgl is not None:
            for _ in bgl:
                pass
        k.barrier()

    if os.environ.get('MIX_STOP') == '1':
        eso.close()
        k.release(mk0)
        return
    with ExitStack() as es:
        wl = WLoader(k, es)
        wret = k.sb("wret", [128, 8, 2048], BF16, es)
        wl.load(wret, lambda c0, c1: wret[:, c0:c1, :], w_ret, 0, 8, 0, 2048)
        tab = [k.sb("tabr", [128, 512], F32, es) for _ in range(2)]
        qs = k.sb("qs", [128, 2, 256], F32, es)
        ks = k.sb("ks", [128, 2, 256], F32, es)
        vb = k.sb("vb", [128, 2, 256], BF16, es)
        gs = k.sb("gs", [128, 2, 256], F32, es)
        t1 = k.sb("t1", [128, 2, 256], F32, es)
        t2 = k.sb("t2", [128, 2, 256], F32, es)
        qb = k.sb("qb", [128, 2, 256], BF16, es)
        kb_ = k.sb("kb", [128, 2, 256], BF16, es)
        qkT = k.sb("qkT", [128, 8, 128], BF16, es)
        PT = k.sb("PT", [128, 2, 128], BF16, es)
        W = k.sb("W", [128, 2, 2, 256], F32, es)
        Tb = k.sb("Tb", [128, 2, 2, 256], BF16, es)
        st = k.sb("st", [128, 2, 6], F32, es)
        mv = k.sb("mv", [128, 2, 2], F32, es)
        rstd = k.sb("rstd", [128, 2], F32, es)
        yn = k.sb("yn", [128, 2, 256], F32, es)
        ycb = k.sb("ycb", [128, 512], BF16, es)
        ycs = k.sb("ycs", [128, 4, 128], BF16, es)
        pq = k.ps("pq", [128, 512], F32, es)
        pk = k.ps("pk", [128, 512], F32, es)
        pv = k.ps("pv", [128, 512], F32, es)
        pg = k.ps("pg", [128, 512], F32, es)
        pTb = k.ps("pTb", [128, 8, 128], BF16, es)
        pS = k.ps("pS", [128, 2, 128], F32, es)
        pO = k.ps("pO", [128, 2, 256], F32, es)
        pD = k.ps("pD", [128, 2, 256], F32, es)
        k.op("pool", lambda e: e.memset(W[:], 0.0), writes=[W])
        k.op("pool", lambda e: e.memset(Tb[:], 0.0), writes=[Tb])
        maskR = cs[:, 128:256]
        qbs = [qb, k.sb("qb2", [128, 2, 256], BF16, es)]
        kbs = [kb_, k.sb("kb2", [128, 2, 256], BF16, es)]
        vbs2 = [vb, k.sb("vb2", [128, 2, 256], BF16, es)]
        gss = [gs, k.sb("gs2", [128, 2, 256], F32, es)]
        bgg = None

        def ret_f(t):
                tb_ = tab[t % 2]
                qb, kb_, vb, gs = qbs[t % 2], kbs[t % 2], vbs2[t % 2], gss[t % 2]
                k.dma("sp", tb_[:], tabR[t], tb_, reads=[tabR], writes=[tb_])
                tok = slice(t * 128, (t + 1) * 128)
                for pi, pb in enumerate((pq, pk, pv, pg)):
                    for c in range(8):
                        k.op("pe", lambda e: e.matmul(pb[:], lhsT=xT[:, c, tok], rhs=wret[:, c, pi * 512:(pi + 1) * 512],
                                                      start=(c == 0), stop=(c == 7)), reads=[xT, wret], writes=[pb])
                for h in range(2):
                    k.op("act", lambda e: e.activation(out=qs[:, h, :], in_=pq[:, h * 256:(h + 1) * 256], func=AF.Copy, scale=decs[:, h:h + 1]),
                         reads=[pq, decs], writes=[qs])
                    k.op("act", lambda e: e.activation(out=ks[:, h, :], in_=pk[:, h * 256:(h + 1) * 256], func=AF.Copy, scale=decs[:, 2 + h:3 + h]),
                         reads=[pk, decs], writes=[ks])
                k.op("act", lambda e: e.activation(out=vb[:].rearrange("p a b -> p (a b)"), in_=pv[:], func=AF.Copy), reads=[pv], writes=[vb])
                k.op("act", lambda e: e.activation(out=gs[:].rearrange("p a b -> p (a b)"), in_=pg[:], func=AF.Silu), reads=[pg], writes=[gs])
                CC = tb_[:, 0:256].rearrange("p (a b) -> p a b", a=2).unsqueeze(1).to_broadcast([128, 2, 2, 128])
                SS0 = tb_[:, 256:384].unsqueeze(1).to_broadcast([128, 2, 128])
                SS1 = tb_[:, 384:512].unsqueeze(1).to_broadcast([128, 2, 128])
                for src, dstb in ((qs, qb), (ks, kb_)):
                    X = src[:].rearrange("p h (a b) -> p h a b", a=2)
                    T1 = t1[:].rearrange("p h (a b) -> p h a b", a=2)
                    T2 = t2[:].rearrange("p h (a b) -> p h a b", a=2)
                    k.op("dve", lambda e: e.tensor_tensor(out=T1, in0=X, in1=CC, op=ALU.mult), reads=[src, tb_], writes=[t1])
                    k.op("pool", lambda e: e.tensor_tensor(out=T2[:, :, 0, :], in0=X[:, :, 1, :], in1=SS0, op=ALU.mult),
                         reads=[src, tb_], writes=[t2])
                    k.op("pool", lambda e: e.tensor_tensor(out=T2[:, :, 1, :], in0=X[:, :, 0, :], in1=SS1, op=ALU.mult),
                         reads=[src, tb_, t2], writes=[t2])
                    k.op("dve", lambda e: e.tensor_tensor(out=dstb[:], in0=t1[:], in1=t2[:], op=ALU.add), reads=[t1, t2], writes=[dstb])

        def ret_g(t):
                tok = slice(t * 128, (t + 1) * 128)
                qb, kb_, vb, gs = qbs[t % 2], kbs[t % 2], vbs2[t % 2], gss[t % 2]
                for h in range(2):
                    for kc in range(2):
                        k.op("pe", lambda e: e.transpose(pTb[:, h * 2 + kc, :], qb[:, h, kc * 128:(kc + 1) * 128], identb[:]),
                             reads=[qb, identb], writes=[pTb])
                        k.op("pe", lambda e: e.transpose(pTb[:, 4 + h * 2 + kc, :], kb_[:, h, kc * 128:(kc + 1) * 128], identb[:]),
                             reads=[kb_, identb], writes=[pTb])
                k.op("act", lambda e: e.activation(out=qkT[:], in_=pTb[:], func=AF.Copy), reads=[pTb], writes=[qkT])
                for h in range(2):
                    for kc in range(2):
                        k.op("pe", lambda e: e.matmul(pS[:, h, :], lhsT=qkT[:, 4 + h * 2 + kc, :], rhs=qkT[:, h * 2 + kc, :],
                                                      start=(kc == 0), stop=(kc == 1)), reads=[qkT], writes=[pS])
                    k.op("dve", lambda e: e.tensor_tensor(out=PT[:, h, :], in0=pS[:, h, :], in1=maskR, op=ALU.mult),
                         reads=[pS, cs], writes=[PT])
                    k.op("pe", lambda e: e.matmul(pO[:, h, :], lhsT=PT[:, h, :], rhs=vb[:, h, :], start=True, stop=False),
                         reads=[PT, vb], writes=[pO])
                    for kc in range(2):
                        k.op("pe", lambda e: e.matmul(pO[:, h, :], lhsT=qkT[:, h * 2 + kc, :], rhs=Tb[:, h, kc, :],
                                                      start=False, stop=(kc == 1)), reads=[qkT, Tb], writes=[pO])
                    for kc in range(2):
                        k.op("pe", lambda e: e.matmul(pD[:, kc, :], lhsT=kb_[:, h, kc * 128:(kc + 1) * 128], rhs=vb[:, h, :],
                                                      start=True, stop=True), reads=[kb_, vb], writes=[pD])
                    k.op("dve", lambda e: e.scalar_tensor_tensor(out=W[:, h].rearrange("p a b -> p (a b)"),
                                                                 in0=W[:, h].rearrange("p a b -> p (a b)"),
                                                                 scalar=decs[:, 4 + h:5 + h],
                                                                 in1=pD[:].rearrange("p a b -> p (a b)"), op0=ALU.mult, op1=ALU.add),
                         reads=[W, decs, pD], writes=[W])
                    k.op("act", lambda e: e.activation(out=Tb[:, h].rearrange("p a b -> p (a b)"), in_=W[:, h].rearrange("p a b -> p (a b)"),
                                                       func=AF.Copy, scale=decs[:, 4 + h:5 + h]), reads=[W, decs], writes=[Tb])
                    k.op("dve", lambda e: e.bn_stats(out=st[:, h, :], in_=pO[:, h, :]), reads=[pO], writes=[st])
                    k.op("dve", lambda e: e.bn_aggr(out=mv[:, h, :], in_=st[:, h, :]), reads=[st], writes=[mv])
                k.op("dve", lambda e: e.tensor_scalar(out=rstd[:], in0=mv[:, :, 1], scalar1=LN_EPS, scalar2=None, op0=ALU.add),
                     reads=[mv], writes=[rstd])
                k.op("act", lambda e: e.activation(out=rstd[:], in_=rstd[:], func=AF.Sqrt), reads=[rstd], writes=[rstd])
                k.op("dve", lambda e: e.reciprocal(out=rstd[:], in_=rstd[:]), reads=[rstd], writes=[rstd])
                for h in range(2):
                    k.op("dve", lambda e: e.tensor_scalar(out=yn[:, h, :], in0=pO[:, h, :], scalar1=mv[:, h, 0:1], scalar2=rstd[:, h:h + 1],
                                                          op0=ALU.subtract, op1=ALU.mult), reads=[pO, mv, rstd], writes=[yn])
                k.op("pool", lambda e: e.tensor_tensor(out=ycb[:], in0=yn[:].rearrange("p a b -> p (a b)"),
                                                       in1=gs[:].rearrange("p a b -> p (a b)"), op=ALU.mult), reads=[yn, gs], writes=[ycb])
                for c in range(4):
                    k.op("pe", lambda e: e.transpose(pTb[:, c, :], ycb[:, c * 128:(c + 1) * 128], identb[:]),
                         reads=[ycb, identb], writes=[pTb])
                k.op("act", lambda e: e.activation(out=ycs[:], in_=pTb[:, 0:4, :], func=AF.Copy), reads=[pTb], writes=[ycs])
                k.dma("sp", ycT.ap[:, tok].rearrange("(c p) t -> p c t", p=128), ycs[:], ycs, reads=[ycs], writes=[ycT])

        ret_f(0)
        for t in range(NT):
            for _ in range(4):
                if bgg is not None and next(bgg, "END") == "END":
                    bgg = None
            if t + 1 < NT:
                ret_f(t + 1)
            ret_g(t)
        if bgg is not None:
            for _ in bgg:
                pass
        k.barrier()

    if os.environ.get('MIX_STOP') == '2':
        eso.close()
        k.release(mk0)
        return
    with ExitStack() as es:
        wl = WLoader(k, es)
        watt = k.sb("watt", [128, 8, 768], BF16, es)
        acc = k.sb("acc", [128, 2, 2, S], F32, es)
        tba = [k.sb("tba", [128, 256], F32, es) for _ in range(2)]
        t1s = [k.sb("a_t1", [128, 4, 128], F32, es) for _ in range(2)]
        t2s = [k.sb("a_t2", [128, 4, 128], F32, es) for _ in range(2)]
        qkbs = [k.sb("qkb", [128, 4, 128], BF16, es) for _ in range(2)]
        vbs = [k.sb("avb", [128, 2, 128], BF16, es) for _ in range(3)]
        qkTs = [k.sb("aqkT", [128, 4, 128], BF16, es) for _ in range(3)]
        Ebs = [k.sb("Eb", [128, 2, 2, 128], BF16, es) for _ in range(2)]
        PTs = [k.sb("PTa", [128, 2, 2, 128], BF16, es) for _ in range(2)]
        pqks = [k.ps("pqk", [128, 512], F32, es) for _ in range(2)]
        pvas = [k.ps("pva", [128, 256], F32, es) for _ in range(2)]
        pTa = k.ps("pTa", [128, 4, 128], BF16, es)
        pSs = [k.ps("pSa", [128, 2, 2, 128], F32, es) for _ in range(2)]
        pN = k.ps("pND", [128, 2, 2, 128], F32, es)
        maskA = cs[:, 256:512].rearrange("p (a b) -> p a b", a=2)
        sc = 128.0 ** -0.5
        blocks = []
        for g, dil in enumerate((1, 4, 16)):
            for r in range(dil):
                for n in range(32 // dil):
                    blocks.append((g, dil, r, n, len(blocks)))

        def tok_of(dil, r, n):
            start = n * 128 * dil + r
            return slice(start, start + 127 * dil + 1, dil)

        def stage_f(blk):
            g, dil, r, n, bi = blk
            tok = tok_of(dil, r, n)
            tb_ = tba[bi % 2]
            t1, t2, qkb = t1s[bi % 2], t2s[bi % 2], qkbs[bi % 2]
            pqk, pva = pqks[bi % 2], pvas[bi % 2]
            vb_ = vbs[bi % 3]
            k.dma("sp", tb_[:], tabA[bi], tb_, reads=[tabA], writes=[tb_])
            for c in range(8):
                k.op("pe", lambda e: e.matmul(pqk[:], lhsT=xT[:, c, tok], rhs=watt[:, c, 0:512], start=(c == 0), stop=(c == 7)),
                     reads=[xT, watt], writes=[pqk])
            for c in range(8):
                k.op("pe", lambda e: e.matmul(pva[:], lhsT=xT[:, c, tok], rhs=watt[:, c, 512:768], start=(c == 0), stop=(c == 7)),
                     reads=[xT, watt], writes=[pva])
            X = pqk[:].rearrange("p (h a b) -> p h a b", h=4, a=2)
            T1 = t1[:].rearrange("p h (a b) -> p h a b", a=2)
            T2 = t2[:].rearrange("p h (a b) -> p h a b", a=2)
            CC = tb_[:, 0:128].rearrange("p (a b) -> p a b", a=2).unsqueeze(1).to_broadcast([128, 4, 2, 64])
            SS0 = tb_[:, 128:192].unsqueeze(1).to_broadcast([128, 4, 64])
            SS1 = tb_[:, 192:256].unsqueeze(1).to_broadcast([128, 4, 64])
            k.op("dve", lambda e: e.tensor_tensor(out=T1, in0=X, in1=CC, op=ALU.mult), reads=[pqk, tb_], writes=[t1])
            k.op("dve", lambda e: e.tensor_tensor(out=T2[:, :, 0, :], in0=X[:, :, 1, :], in1=SS0, op=ALU.mult),
                 reads=[pqk, tb_], writes=[t2])
            k.op("dve", lambda e: e.tensor_tensor(out=T2[:, :, 1, :], in0=X[:, :, 0, :], in1=SS1, op=ALU.mult),
                 reads=[pqk, tb_, t2], writes=[t2], nowaw=True)
            k.op("pool", lambda e: e.tensor_tensor(out=qkb[:], in0=t1[:], in1=t2[:], op=ALU.add), reads=[t1, t2], writes=[qkb])
            k.op("act", lambda e: e.activation(out=vb_[:].rearrange("p a b -> p (a b)"), in_=pva[:], func=AF.Copy),
                 reads=[pva], writes=[vb_])

        def stage_g(blk):
            g, dil, r, n, bi = blk
            tok = tok_of(dil, r, n)
            qkb = qkbs[bi % 2]
            qc, qp = qkTs[bi % 3], qkTs[(bi - 1) % 3]
            vc, vp = vbs[bi % 3], vbs[(bi - 1) % 3]
            pS, eb, pt = pSs[bi % 2], Ebs[bi % 2], PTs[bi % 2]
            lo = 0 if n > 0 else 1
            for j in range(4):
                k.op("pe", lambda e: e.transpose(pTa[:, j, :], qkb[:, j, :], identb[:]), reads=[qkb, identb], writes=[pTa])
            k.op("act", lambda e: e.activation(out=qc[:], in_=pTa[:], func=AF.Copy), reads=[pTa], writes=[qc])
            for hh in range(2):
                if n > 0:
                    k.op("pe", lambda e: e.matmul(pS[:, hh, 0, :], lhsT=qp[:, 2 + hh, :], rhs=qc[:, hh, :], start=True, stop=True),
                         reads=[qp, qc], writes=[pS])
                k.op("pe", lambda e: e.matmul(pS[:, hh, 1, :], lhsT=qc[:, 2 + hh, :], rhs=qc[:, hh, :], start=True, stop=True),
                     reads=[qc], writes=[pS])
            k.op("act", lambda e: e.activation(out=eb[:, :, lo:2, :], in_=pS[:, :, lo:2, :], func=AF.Exp, scale=sc),
                 reads=[pS], writes=[eb])
            mk_ = maskA[:, lo:2, :].unsqueeze(1).to_broadcast([128, 2, 2 - lo, 128])
            k.op("pool", lambda e: e.tensor_tensor(out=pt[:, :, lo:2, :], in0=eb[:, :, lo:2, :], in1=mk_, op=ALU.mult),
                 reads=[eb, cs], writes=[pt])
            for hh in range(2):
                for which in range(2):
                    lp = vp[:, hh, :] if which == 0 else onesb[:]
                    lc = vc[:, hh, :] if which == 0 else onesb[:]
                    if n > 0:
                        k.op("pe", lambda e: e.matmul(pN[:, hh, which, :], lhsT=lp, rhs=pt[:, hh, 0, :], start=True, stop=False),
                             reads=[vp, onesb, pt], writes=[pN])
                    k.op("pe", lambda e: e.matmul(pN[:, hh, which, :], lhsT=lc, rhs=pt[:, hh, 1, :], start=(n == 0), stop=True),
                         reads=[vc, onesb, pt], writes=[pN])
            av = acc[:, :, :, tok]
            if g == 0:
                k.op("dve", lambda e: e.tensor_copy(out=av, in_=pN[:]), reads=[pN], writes=[acc])
            else:
                k.op("dve", lambda e: e.tensor_tensor(out=av, in0=pN[:], in1=av, op=ALU.add), reads=[pN, acc], writes=[acc])

        def load_watt(g):
            wl.load(watt, lambda c0, c1: watt[:, c0:c1, :], w_att, 0, 8, g * 768, 768)

        load_watt(0)
        stage_f(blocks[0])
        for i, blk in enumerate(blocks):
            if i + 1 < len(blocks):
                nb_ = blocks[i + 1]
                if nb_[0] != blk[0]:
                    load_watt(nb_[0])
                stage_f(nb_)
            stage_g(blk)
        rd = k.sb("rd", [128, 1024], F32, es)
        yas = k.sb("yas", [128, 1024], BF16, es)
        for hh in range(2):
            for q4 in range(4):
                ts_ = slice(q4 * 1024, (q4 + 1) * 1024)
                k.op("dve", lambda e: e.reciprocal(out=rd[:], in_=acc[:, hh, 1, ts_]), reads=[acc], writes=[rd])
                k.op("dve", lambda e: e.tensor_tensor(out=yas[:], in0=acc[:, hh, 0, ts_], in1=rd[:], op=ALU.mult), reads=[acc, rd], writes=[yas])
                k.dma("sp", yaT[hh * 128:(hh + 1) * 128, ts_], yas[:], yas, reads=[yas], writes=[yaT])
        k.barrier()

    if os.environ.get('MIX_STOP') == '3':
        eso.close()
        k.release(mk0)
        return
    with ExitStack() as es:
        wl = WLoader(k, es, n=1)
        wbr = k.sb("wbr", [128, 10, 1024], BF16, es)
        wout = k.sb("wout", [128, 8, 1024], BF16, es)
        for c0 in range(0, 10, 2):
            wl.load(wbr, lambda a, b, c0=c0: wbr[:, c0 + a:c0 + b, :], w_br, c0 * 128, 2, 0, 1024)
        for c0 in range(0, 8, 2):
            wl.load(wout, lambda a, b, c0=c0: wout[:, c0 + a:c0 + b, :], w_out, c0 * 128, 2, 0, 1024)
        ya_s = k.sb("ya_s", [128, 2, 512], BF16, es)
        yb_s = k.sb("yb_s", [128, 4, 512], BF16, es)
        yc_s = k.sb("yc_s", [128, 4, 512], BF16, es)
        sgb = [k.sb("sgb", [128, 24, 512], BF16, es) for _ in range(2)]
        u1s = [k.sb("u1", [128, 512], F32, es) for _ in range(2)]
        u2s = [k.sb("u2", [128, 512], F32, es) for _ in range(2)]
        mT = k.sb("mT", [128, 8, 512], BF16, es)
        mo = [k.sb("mo", [128, 1024], F32, es) for _ in range(2)]
        PPs = [[k.ps("PP", [128, 512], F32, es) for _ in range(3)] for _ in range(2)]
        po = [k.ps("po", [128, 512], F32, es) for _ in range(2)]
        it = 0
        oi = 0
        for tg in range(8):
            tok = slice(tg * 512, (tg + 1) * 512)
            sg = sgb[tg % 2]
            k.dma("sp", sg[:], sgT.ap[:, tok].rearrange("(c p) t -> p c t", p=128), sg, reads=[sgT], writes=[sg])
            k.dma("sp", ya_s[:], yaT.ap[:, tok].rearrange("(c p) t -> p c t", p=128), ya_s, reads=[yaT], writes=[ya_s])
            k.dma("sp", yb_s[:], ybT.ap[:, tok].rearrange("(c p) t -> p c t", p=128), yb_s, reads=[ybT], writes=[yb_s])
            k.dma("sp", yc_s[:], ycT.ap[:, tok].rearrange("(c p) t -> p c t", p=128), yc_s, reads=[ycT], writes=[yc_s])
            for mc in range(8):
                PP = PPs[it % 2]
                u1, u2 = u1s[it % 2], u2s[it % 2]
                it += 1
                msl = slice(mc * 128, (mc + 1) * 128)
                for kc in range(2):
                    k.op("pe", lambda e: e.matmul(PP[0][:], lhsT=wbr[:, kc, msl], rhs=ya_s[:, kc, :], start=(kc == 0), stop=(kc == 1)),
                         reads=[wbr, ya_s], writes=[PP[0]])
                for kc in range(4):
                    k.op("pe", lambda e: e.matmul(PP[1][:], lhsT=wbr[:, 2 + kc, msl], rhs=yb_s[:, kc, :], start=(kc == 0), stop=(kc == 3)),
                         reads=[wbr, yb_s], writes=[PP[1]])
                for kc in range(4):
                    k.op("pe", lambda e: e.matmul(PP[2][:], lhsT=wbr[:, 6 + kc, msl], rhs=yc_s[:, kc, :], start=(kc == 0), stop=(kc == 3)),
                         reads=[wbr, yc_s], writes=[PP[2]])
                k.op("dve", lambda e: e.tensor_tensor(out=u1[:], in0=PP[0][:], in1=sg[:, mc, :], op=ALU.mult), reads=[PP[0], sg], writes=[u1])
                k.op("dve", lambda e: e.tensor_tensor(out=u2[:], in0=PP[1][:], in1=sg[:, 8 + mc, :], op=ALU.mult), reads=[PP[1], sg], writes=[u2])
                k.op("dve", lambda e: e.tensor_tensor(out=u1[:], in0=u1[:], in1=u2[:], op=ALU.add), reads=[u1, u2], writes=[u1])
                k.op("dve", lambda e: e.tensor_tensor(out=u2[:], in0=PP[2][:], in1=sg[:, 16 + mc, :], op=ALU.mult), reads=[PP[2], sg], writes=[u2])
                k.op("pool", lambda e: e.tensor_tensor(out=mT[:, mc, :], in0=u1[:], in1=u2[:], op=ALU.add), reads=[u1, u2], writes=[mT])
            for tt_ in range(4):
                mob = mo[oi % 2]
                oi += 1
                for half in range(2):
                    for mc in range(8):
                        k.op("pe", lambda e: e.matmul(po[half][:], lhsT=mT[:, mc, tt_ * 128:(tt_ + 1) * 128],
                                                      rhs=wout[:, mc, half * 512:(half + 1) * 512], start=(mc == 0), stop=(mc == 7)),
                             reads=[mT, wout], writes=[po[half]])
                    k.op("act", lambda e: e.activation(out=mob[:, half * 512:(half + 1) * 512], in_=po[half][:], func=AF.Copy),
                         reads=[po[half]], writes=[mob])
                r0 = tg * 512 + tt_ * 128
                k.dma("pool", m_part[r0:r0 + 128, :], mob[:], mob, reads=[mob], writes=[m_part])
        k.barrier()
    eso.close()
    k.release(mk0)


def _mixer_drams(k, debug=False, nl=1):
    EI = "ExternalInput"
    T = {}
    T["w_att"] = k.dram("w_att", [nl, D, 2304], F32, EI)
    T["w_lru"] = k.dram("w_lru", [nl, D, 1024], F32, EI)
    T["w_ret"] = k.dram("w_ret", [nl, D, 2048], F32, EI)
    T["w_gate"] = k.dram("w_gate", [nl, D, 3072], F32, EI)
    T["w_br"] = k.dram("w_br", [nl, 1280, D], F32, EI)
    T["w_out"] = k.dram("w_out", [nl, D, D], F32, EI)
    T["lru_vec"] = k.dram("lru_vec", [nl, 512, 8], F32, EI)
    T["wa_bd"] = k.dram("wa_bd", [nl, 512, 128], F32, EI)
    T["wx_bd"] = k.dram("wx_bd", [nl, 512, 128], F32, EI)
    T["dec"] = k.dram("dec", [nl, 128, 6], F32, EI)
    T["tabA"] = k.dram("tabA", [96, 128, 256], F32, EI)
    T["tabR"] = k.dram("tabR", [32, 128, 512], F32, EI)
    T["cst"] = k.dram("cst", [128, 512], F32, EI)
    kindS = "ExternalOutput" if debug else "Internal"
    T["yaT"] = k.dram("yaT", [256, S], BF16, kindS)
    T["ybT"] = k.dram("ybT", [512, S], BF16, kindS)
    T["ycT"] = k.dram("ycT", [512, S], BF16, kindS)
    T["sgT"] = k.dram("sgT", [3072, S], BF16)
    return T


PER_PASS = ("w_att", "w_lru", "w_ret", "w_gate", "w_br", "w_out", "lru_vec", "wa_bd", "wx_bd", "dec")


def _mixer_views(k, T, i):
    V = dict(T)
    for n in PER_PASS:
        V[n] = k.view(T[n], T[n].ap[i])
    return V


def build_mixer(debug=False):
    nc = bass.Bass("TRN2", target_bir_lowering=False)
    k = KB(nc)
    T = _mixer_drams(k, debug, 1)
    T["x"] = k.dram("x", [S, D], F32, "ExternalInput")
    T["m_part"] = k.dram("m_part", [S, D], F32, "ExternalOutput")
    mixer_body(k, _mixer_views(k, T, 0))
    k.finish()
    return nc


def _consts_mixer():
    i = np.arange(128)
    ident = np.eye(128, dtype=np.float32)
    maskR = (i[:, None] <= i[None, :]).astype(np.float32)
    maskP = (i[:, None] >= i[None, :]).astype(np.float32)
    maskC = (i[:, None] <= i[None, :]).astype(np.float32)
    cst = np.concatenate([ident, maskR, maskP, maskC], axis=1).astype(np.float32)
    pos = np.arange(S, dtype=np.float32)
    invA = (10000.0 ** (-np.arange(0, 128, 2, dtype=np.float32) / 128)).astype(np.float32)
    angA = pos[:, None] * invA[None, :]
    cA, sA = np.cos(angA).astype(np.float32), np.sin(angA).astype(np.float32)
    tabA = np.zeros((96, 128, 256), np.float32)
    bi = 0
    for dil in (1, 4, 16):
        nb = 32 // dil
        for r in range(dil):
            for n in range(nb):
                tk = (n * 128 + i) * dil + r
                tabA[bi] = np.concatenate([cA[tk], cA[tk], -sA[tk], sA[tk]], axis=1)
                bi += 1
    invR = (10000.0 ** (-np.arange(0, 256, 2, dtype=np.float32) / 256)).astype(np.float32)
    angR = pos[:, None] * invR[None, :]
    cR, sR = np.cos(angR).astype(np.float32), np.sin(angR).astype(np.float32)
    tabR = np.concatenate([cR, cR, -sR, sR], axis=1).reshape(32, 128, 512).astype(np.float32)
    return cst, tabA, tabR


def _dec(c):
    i = np.arange(128, dtype=np.float64)
    out = np.zeros((128, 6), np.float64)
    for hh in range(2):
        h = 2 * c + hh
        gam = 1.0 - 2.0 ** (-5.0 - h)
        out[:, hh] = gam ** i
        out[:, 2 + hh] = gam ** (-i) * 256 ** -0.5
        out[:, 4 + hh] = gam ** 128
    return out.astype(np.float32)


def _mixer_inputs(l, xb, c, inp, consts):
    cst, tabA, tabR = consts
    w_in = inp["w_in"][l]
    cols = []
    for g in range(3):
        for base in (0, 1536, 3072):
            for hh in range(2):
                hd = g * 4 + 2 * c + hh
                cols.append(np.arange(base + hd * 128, base + (hd + 1) * 128))
    w_att = np.ascontiguousarray(w_in[:, np.concatenate(cols)])
    w_lru = np.ascontiguousarray(np.concatenate([w_in[:, 4608 + c * 512: 4608 + (c + 1) * 512],
                                                 w_in[:, 5632 + c * 512: 5632 + (c + 1) * 512]], axis=1))
    w_ret = np.ascontiguousarray(np.concatenate([w_in[:, b0 + c * 512: b0 + (c + 1) * 512] for b0 in (6656, 7680, 8704, 9728)], axis=1))
    w_gate = np.ascontiguousarray(w_in[:, 10752:13824])
    wb = inp["w_branch"][l]
    w_br = np.ascontiguousarray(np.concatenate([wb[c * 256:(c + 1) * 256], wb[512 + c * 512: 512 + (c + 1) * 512],
                                                wb[1536 + c * 512: 1536 + (c + 1) * 512]], axis=0))
    sl = slice(c * 512, (c + 1) * 512)
    lru_vec = np.ascontiguousarray(np.stack([inp["conv_w"][l][0, sl], inp["conv_w"][l][1, sl], inp["conv_w"][l][2, sl],
                                             inp["conv_w"][l][3, sl], inp["conv_b"][l][sl], inp["lru_ba"][l][sl],
                                             inp["lru_bx"][l][sl], inp["lru_lambda"][l][sl]], axis=1).astype(np.float32))

    def bd(w):
        o = np.zeros((4, 128, 128), np.float32)
        for ch in range(4):
            for j in range(2):
                o[ch, j * 64:(j + 1) * 64, j * 64:(j + 1) * 64] = w[c * 8 + ch * 2 + j]
        return o.reshape(512, 128)

    d = {"w_att": w_att, "w_lru": w_lru, "w_ret": w_ret, "w_gate": w_gate, "w_br": w_br,
         "w_out": np.ascontiguousarray(inp["w_out"][l]), "lru_vec": lru_vec, "wa_bd": bd(inp["lru_wa"][l]),
         "wx_bd": bd(inp["lru_wx"][l]), "dec": _dec(c)}
    d = {k_: v[None] for k_, v in d.items()}
    if xb is not None:
        d.update({"x": xb, "tabA": tabA, "tabR": tabR, "cst": cst})
    return d


def _fused_inputs(xb, inp, consts, cstp):
    cst, tabA, tabR = consts
    per = [_mixer_inputs(l, None, c, inp, consts) for l in range(DEPTH) for c in range(2)]
    d = {n: np.ascontiguousarray(np.concatenate([p[n] for p in per], axis=0)) for n in PER_PASS}
    d.update({"x": xb, "tabA": tabA, "tabR": tabR, "cst": cst, "cstp": cstp})
    d["lnp"] = np.ascontiguousarray(np.stack([np.stack([inp["ln1_g"][l], inp["ln1_b"][l], inp["ln2_g"][l], inp["ln2_b"][l]])
                                               for l in range(DEPTH)]).astype(np.float32))
    d["wq"] = np.ascontiguousarray(inp["peer_wq"])
    d["kk"] = np.ascontiguousarray(np.stack([inp["peer_k1"], inp["peer_k2"]], axis=2).reshape(DEPTH, 16, 128, 128))
    for l in range(DEPTH):
        d[f"U{l}"] = np.ascontiguousarray(inp["peer_u"][l])
        d[f"V{l}"] = np.ascontiguousarray(inp["peer_v"][l])
    return d


TP = 2048
NTP = TP // 128
NG = 14


def _layer_norm(k, src, dst, gt, bt, st, mv, rstd):
    for j in range(2):
        k.op("dve", lambda e: e.bn_stats(out=st[:, j, :], in_=src[:, j * 512:(j + 1) * 512]), reads=[src], writes=[st])
    k.op("dve", lambda e: e.bn_aggr(out=mv[:], in_=st[:].rearrange("p a b -> p (a b)")), reads=[st], writes=[mv])
    k.op("dve", lambda e: e.tensor_scalar(out=rstd[:], in0=mv[:, 1:2], scalar1=LN_EPS, scalar2=None, op0=ALU.add),
         reads=[mv], writes=[rstd])
    k.op("act", lambda e: e.activation(out=rstd[:], in_=rstd[:], func=AF.Sqrt), reads=[rstd], writes=[rstd])
    k.op("dve", lambda e: e.reciprocal(out=rstd[:], in_=rstd[:]), reads=[rstd], writes=[rstd])
    k.op("dve", lambda e: e.tensor_scalar(out=mv[:, 1:2], in0=mv[:, 0:1], scalar1=rstd[:, 0:1], scalar2=-1.0,
                                          op0=ALU.mult, op1=ALU.mult), reads=[mv, rstd], writes=[mv])
    k.op("act", lambda e: e.activation(out=dst[:], in_=src[:], func=AF.Identity, scale=rstd[:, 0:1], bias=mv[:, 1:2]),
         reads=[src, mv, rstd], writes=[dst])
    k.op("pool", lambda e: e.tensor_tensor(out=dst[:], in0=dst[:], in1=gt[:], op=ALU.mult), reads=[dst, gt], writes=[dst])
    k.op("pool", lambda e: e.tensor_tensor(out=dst[:], in0=dst[:], in1=bt[:], op=ALU.add), reads=[dst, bt], writes=[dst])


def uv_convert_gen(k, U, V, UV, es0, engines):
    NB_ = 4
    cvs = [k.sb("cvs", [128, 2, D], F32, es0) for _ in range(NB_)]
    cvb = [k.sb("cvb", [128, 2, D], BF16, es0) for _ in range(NB_)]
    ci_ = 0
    for si_, src_ in enumerate((U, V)):
        for ch in range(64):
            a_, b_ = cvs[ci_ % NB_], cvb[ci_ % NB_]
            eng = engines[ci_ % len(engines)]
            ci_ += 1
            rs_ = slice(ch * 256, (ch + 1) * 256)
            k.dma("sp", a_[:], src_.ap[rs_, :].rearrange("(p j) d -> p j d", j=2), a_, reads=[src_], writes=[a_])
            if eng == "act":
                k.op("act", lambda e: e.activation(out=b_[:], in_=a_[:], func=AF.Copy), reads=[a_], writes=[b_])
            else:
                k.op(eng, lambda e: e.tensor_copy(out=b_[:], in_=a_[:]), reads=[a_], writes=[b_])
            k.dma("pool", UV.ap[rs_, si_ * D:(si_ + 1) * D].rearrange("(p j) d -> p j d", j=2), b_[:], b_, reads=[b_], writes=[UV])
            yield None


def peer_body(k, T, ntiles):
    x, m0, m1, lnp, wq, kk, U, V, cstp, xo = (T[n] for n in ("x", "m0", "m1", "lnp", "wq", "kk", "U", "V", "cstp", "xo"))
    mk0 = k.mark()

    UV = T["UV"]
    if not T.get("UV_ready"):
        with ExitStack() as es0:
            for _ in uv_convert_gen(k, U, V, UV, es0, ("act", "dve")):
                pass
            k.barrier()
    es = ExitStack()
    cs = k.sb("cs", [128, 256], F32, es)
    k.dma("sp", cs[:], cstp[:], cs, reads=[cstp], writes=[cs])
    ident = cs
    iota16 = cs[:, 128:144]
    lnt = [k.sb("lnt", [128, D], F32, es) for _ in range(4)]
    for j in range(4):
        k.dma("sp", lnt[j][:], lnp.ap[j].partition_broadcast(128), lnt[j], reads=[lnp], writes=[lnt[j]])
    es_s = ExitStack()
    wl = WLoader(k, es_s, side="right")
    wqb = k.sb("wqb", [128, 8, 2048], BF16, es)
    wl.load(wqb, lambda c0, c1: wqb[:, c0:c1, :], wq, 0, 8, 0, 2048)
    kks = k.sb("kks", [128, 16, 128], F32, es_s, side="right")
    kkT = k.sb("kkT", [128, 16, 128], BF16, es)
    k.dma("sp", kks[:], kk.ap.rearrange("h k c -> k h c"), kks, reads=[kk], writes=[kks])
    pT = k.ps("pT", [128, 8, 128], F32, es)
    for half in range(2):
        for j in range(8):
            k.op("pe", lambda e: e.transpose(pT[:, j, :], kks[:, half * 8 + j, :], ident[:, 0:128]), reads=[kks, cs], writes=[pT])
        k.op("dve", lambda e: e.tensor_copy(out=kkT[:, half * 8:(half + 1) * 8, :], in_=pT[:]), reads=[pT], writes=[kkT])

    xt = k.sb("xt", [128, D], F32, es)
    m0t = k.sb("m0t", [128, D], F32, es)
    m1t = k.sb("m1t", [128, D], F32, es)
    x1 = k.sb("x1", [128, D], F32, es)
    x1T = k.sb("x1T", [128, 8, 128], BF16, es)
    qT = k.sb("qT", [128, 16, 128], BF16, es)
    sc = k.sb("sc", [128, 16, 128], F32, es)
    sc2 = k.sb("sc2", [128, 16, 128], F32, es)
    v16 = k.sb("v16", [128, 16, 16], F32, es)
    i16 = k.sb("i16", [128, 16, 16], U32, es)
    i16f = k.sb("i16f", [128, 16, 16], F32, es)
    cand = k.sb("cand", [128, 8, 256], F32, es)
    cand2 = k.sb("cand2", [128, 8, 256], F32, es)
    cv = k.sb("cv", [128, 8, 16], F32, es)
    ci = k.sb("ci", [128, 8, 16], U32, es)
    hi_u = k.sb("hi_u", [128, 8, 16], U32, es)
    lo_u = k.sb("lo_u", [128, 8, 16], U32, es)
    hi_f = k.sb("hi_f", [128, 8, 16], F32, es)
    lo_f = k.sb("lo_f", [128, 8, 16], F32, es)
    eg = k.sb("eg", [128, 8, 16], F32, es)
    sm = k.sb("sm", [128, 8], F32, es)
    gg = k.sb("gg", [128, 128], F32, es)
    eq = k.sb("eq", [128, 8, 16, 16], F32, es)
    asel = k.sb("asel", [128, 8, 16], F32, es)
    bsel = k.sb("bsel", [128, 8, 16], F32, es)
    eidf = k.sb("eidf", [128, 128], F32, es)
    eid = k.sb("eid", [128, 128], U32, es)
    junk = k.sb("junk", [128, D], F32, es)
    yacc = k.sb("yacc", [128, D], F32, es)
    NR = 8
    pre_r = [k.sb("pre_r", [128, 1], F32, es) for _ in range(NR)]
    gl_r = [k.sb("gl_r", [128, 1], F32, es) for _ in range(NR)]
    dg_r = [k.sb("dg_r", [128, 128], BF16, es) for _ in range(NR)]
    identb = k.sb("identb", [128, 128], BF16, es)
    k.op("dve", lambda e: e.tensor_copy(out=identb[:], in_=cs[:, 0:128]), reads=[cs], writes=[identb])
    st = k.sb("st", [128, 2, 6], F32, es)
    mv = k.sb("mv", [128, 2], F32, es)
    rstd = k.sb("rstd", [128, 1], F32, es)
    k.barrier()
    es_s.close()
    gb = [k.sb("gb", [128, 2 * D], BF16, es) for _ in range(NG)]
    py = k.ps("py", [128, D], F32, es)
    pq = [k.ps("pq", [128, 4, 128], F32, es) for _ in range(2)]
    psc = [k.ps("psc", [128, 4, 128], F32, es) for _ in range(2)]
    x1p = [x1, k.sb("x1b", [128, D], F32, es)]
    ggp = [gg, k.sb("ggb", [128, 128], F32, es)]
    eidp = [eid, k.sb("eidb", [128, 128], U32, es)]
    ot = k.sb("ot", [128, D], F32, es)
    st2 = k.sb("st2", [128, 2, 6], F32, es)
    mv2 = k.sb("mv2", [128, 2], F32, es)
    rstd2 = k.sb("rstd2", [128, 1], F32, es)
    nt_run = int(os.environ.get("PEER_TILES", ntiles))

    def route_gen(t, par):
            rows = slice(t * 128, (t + 1) * 128)
            yield k.dma("sp", xt[:], x[rows, :], xt, reads=[x], writes=[xt])
            yield k.dma("sp", m0t[:], m0[rows, :], m0t, reads=[m0], writes=[m0t])
            yield k.dma("sp", m1t[:], m1[rows, :], m1t, reads=[m1], writes=[m1t])
            yield k.op("dve", lambda e: e.scalar_tensor_tensor(out=xt[:], in0=xt[:], scalar=float(ALPHA), in1=m0t[:], op0=ALU.mult, op1=ALU.add),
                 reads=[xt, m0t], writes=[xt])
            yield k.op("dve", lambda e: e.tensor_tensor(out=xt[:], in0=xt[:], in1=m1t[:], op=ALU.add), reads=[xt, m1t], writes=[xt])
            _layer_norm(k, xt, x1p[par], lnt[0], lnt[1], st, mv, rstd)
            yield None
            for c in range(8):
                yield k.op("pe", lambda e: e.transpose(pT[:, c, :], x1p[par][:, c * 128:(c + 1) * 128], ident[:, 0:128]), reads=[x1p[par], cs], writes=[pT])
            yield k.op("act", lambda e: e.activation(out=x1T[:], in_=pT[:], func=AF.Copy), reads=[pT], writes=[x1T])
            for q4 in range(4):
                pb = pq[q4 % 2]
                for j in range(4):
                    hc = q4 * 4 + j
                    for c in range(8):
                        yield k.op("pe", lambda e: e.matmul(pb[:, j, :], lhsT=wqb[:, c, hc * 128:(hc + 1) * 128], rhs=x1T[:, c, :],
                                                      start=(c == 0), stop=(c == 7)), reads=[wqb, x1T], writes=[pb])
                yield k.op("act", lambda e: e.activation(out=qT[:, q4 * 4:(q4 + 1) * 4, :], in_=pb[:], func=AF.Copy), reads=[pb], writes=[qT])
            for q4 in range(4):
                pb = psc[q4 % 2]
                for j in range(4):
                    hc = q4 * 4 + j
                    yield k.op("pe", lambda e: e.matmul(pb[:, j, :], lhsT=qT[:, hc, :], rhs=kkT[:, hc, :], start=True, stop=True),
                         reads=[qT, kkT], writes=[pb])
                yield k.op("act", lambda e: e.activation(out=sc[:, q4 * 4:(q4 + 1) * 4, :], in_=pb[:], func=AF.Copy), reads=[pb], writes=[sc])
            for hc in range(16):
                yield k.op("dve", lambda e: e.max(out=v16[:, hc, 0:8], in_=sc[:, hc, :]), reads=[sc], writes=[v16], nowaw=True)
            for hc in range(16):
                yield k.op("dve", lambda e: e.max_index(out=i16[:, hc, 0:8], in_max=v16[:, hc, 0:8], in_values=sc[:, hc, :]),
                     reads=[sc, v16], writes=[i16], nowaw=True)
            for hc in range(16):
                yield k.op("dve", lambda e: e.match_replace(out=sc2[:, hc, :], in_to_replace=v16[:, hc, 0:8], in_values=sc[:, hc, :], imm_value=-1e30),
                     reads=[sc, v16], writes=[sc2], nowaw=True)
            for hc in range(16):
                yield k.op("dve", lambda e: e.max(out=v16[:, hc, 8:16], in_=sc2[:, hc, :]), reads=[sc2], writes=[v16], nowaw=True)
            for hc in range(16):
                yield k.op("dve", lambda e: e.max_index(out=i16[:, hc, 8:16], in_max=v16[:, hc, 8:16], in_values=sc2[:, hc, :]),
                     reads=[sc2, v16], writes=[i16], nowaw=True)
            vv = v16[:].rearrange("p (h s) i -> p h s i", s=2)
            yield k.op("pool", lambda e: e.tensor_tensor(out=cand[:].rearrange("p h (i j) -> p h i j", i=16),
                                                  in0=vv[:, :, 0, :].unsqueeze(3).to_broadcast([128, 8, 16, 16]),
                                                  in1=vv[:, :, 1, :].unsqueeze(2).to_broadcast([128, 8, 16, 16]), op=ALU.add),
                 reads=[v16], writes=[cand])
            for h in range(8):
                yield k.op("dve", lambda e: e.max(out=cv[:, h, 0:8], in_=cand[:, h, :]), reads=[cand], writes=[cv], nowaw=True)
            for h in range(8):
                yield k.op("dve", lambda e: e.max_index(out=ci[:, h, 0:8], in_max=cv[:, h, 0:8], in_values=cand[:, h, :]),
                     reads=[cand, cv], writes=[ci], nowaw=True)
            for h in range(8):
                yield k.op("dve", lambda e: e.match_replace(out=cand2[:, h, :], in_to_replace=cv[:, h, 0:8], in_values=cand[:, h, :], imm_value=-1e30),
                     reads=[cand, cv], writes=[cand2], nowaw=True)
            for h in range(8):
                yield k.op("dve", lambda e: e.max(out=cv[:, h, 8:16], in_=cand2[:, h, :]), reads=[cand2], writes=[cv], nowaw=True)
            for h in range(8):
                yield k.op("dve", lambda e: e.max_index(out=ci[:, h, 8:16], in_max=cv[:, h, 8:16], in_values=cand2[:, h, :]),
                     reads=[cand2, cv], writes=[ci], nowaw=True)
            yield k.op("dve", lambda e: e.tensor_tensor(out=eg[:], in0=cv[:], in1=cv[:, :, 0:1].to_broadcast([128, 8, 16]), op=ALU.subtract),
                 reads=[cv], writes=[eg])
            yield k.op("act", lambda e: e.activation(out=eg[:], in_=eg[:], func=AF.Exp), reads=[eg], writes=[eg])
            yield k.op("dve", lambda e: e.tensor_reduce(out=sm[:], in_=eg[:], axis=AX.X, op=ALU.add), reads=[eg], writes=[sm])
            yield k.op("dve", lambda e: e.reciprocal(out=sm[:], in_=sm[:]), reads=[sm], writes=[sm])
            yield k.op("dve", lambda e: e.tensor_tensor(out=ggp[par][:].rearrange("p (h k) -> p h k", h=8), in0=eg[:],
                                                  in1=sm[:].unsqueeze(2).to_broadcast([128, 8, 16]), op=ALU.mult),
                 reads=[eg, sm], writes=[ggp[par]])
            yield k.op("dve", lambda e: e.tensor_single_scalar(out=hi_u[:], in_=ci[:], scalar=4, op=ALU.logical_shift_right), reads=[ci], writes=[hi_u])
            yield k.op("dve", lambda e: e.tensor_single_scalar(out=lo_u[:], in_=ci[:], scalar=15, op=ALU.bitwise_and), reads=[ci], writes=[lo_u])
            yield k.op("dve", lambda e: e.tensor_copy(out=hi_f[:], in_=hi_u[:]), reads=[hi_u], writes=[hi_f])
            yield k.op("dve", lambda e: e.tensor_copy(out=lo_f[:], in_=lo_u[:]), reads=[lo_u], writes=[lo_f])
            yield k.op("dve", lambda e: e.tensor_copy(out=i16f[:], in_=i16[:]), reads=[i16], writes=[i16f])
            iv = i16f[:].rearrange("p (h s) i -> p h s i", s=2)
            io = iota16.unsqueeze(1).unsqueeze(1).to_broadcast([128, 8, 16, 16])
            for sel, src, dsts in ((0, hi_f, asel), (1, lo_f, bsel)):
                yield k.op("dve", lambda e: e.tensor_tensor(out=eq[:], in0=src[:].unsqueeze(3).to_broadcast([128, 8, 16, 16]), in1=io, op=ALU.is_equal),
                     reads=[src, cs], writes=[eq])
                yield k.op("pool", lambda e: e.tensor_tensor(out=eq[:], in0=eq[:], in1=iv[:, :, sel, :].unsqueeze(2).to_broadcast([128, 8, 16, 16]),
                                                      op=ALU.mult), reads=[eq, i16f], writes=[eq])
                yield k.op("dve", lambda e: e.tensor_reduce(out=dsts[:], in_=eq[:], axis=AX.X, op=ALU.add), reads=[eq], writes=[dsts])
            yield k.op("dve", lambda e: e.scalar_tensor_tensor(out=eidf[:], in0=asel[:].rearrange("p h k -> p (h k)"), scalar=128.0,
                                                         in1=bsel[:].rearrange("p h k -> p (h k)"), op0=ALU.mult, op1=ALU.add),
                 reads=[asel, bsel], writes=[eidf])
            yield k.op("dve", lambda e: e.tensor_copy(out=eidp[par][:], in_=eidf[:]), reads=[eidf], writes=[eidp[par]])

    gi = 0
    for _ in route_gen(0, 0):
        pass
    for t in range(nt_run):
        par = t % 2
        rows = slice(t * 128, (t + 1) * 128)
        nxt = route_gen(t + 1, 1 - par) if t + 1 < nt_run else None
        for hk in range(128):
            g_ = gb[gi % NG]
            pr_, gl_, dg_ = pre_r[gi % NR], gl_r[gi % NR], dg_r[gi % NR]
            gi += 1
            k.dma("pool", g_[:], UV[:, :], g_, reads=[UV, eidp[par]], writes=[g_],
                  indirect=bass.IndirectOffsetOnAxis(ap=eidp[par][:, hk:hk + 1], axis=0))
            k.op("dve", lambda e: e.scalar_tensor_tensor(out=junk[:], in0=g_[:, 0:D], scalar=1.0, in1=x1p[par][:],
                                                         op0=ALU.mult, op1=ALU.mult, accum_out=pr_[:]),
                 reads=[g_, x1p[par]], writes=[pr_])
            k.op("act", lambda e: e.activation(out=gl_[:], in_=pr_[:], func=AF.Gelu), reads=[pr_], writes=[gl_])
            k.op("act", lambda e: e.activation(out=gl_[:], in_=gl_[:], func=AF.Copy, scale=ggp[par][:, hk:hk + 1]),
                 reads=[gl_, ggp[par]], writes=[gl_])
            k.op("act", lambda e: e.activation(out=dg_[:], in_=identb[:], func=AF.Copy, scale=gl_[:, 0:1]),
                 reads=[identb, gl_], writes=[dg_])
            for half in range(2):
                k.op("pe", lambda e: e.matmul(py[:, half * 512:(half + 1) * 512], lhsT=dg_[:], rhs=g_[:, D + half * 512: D + (half + 1) * 512],
                                              start=(hk == 0), stop=(hk == 127)), reads=[dg_, g_], writes=[py])
            for _ in range(3):
                if nxt is not None and next(nxt, "END") == "END":
                    nxt = None
        if nxt is not None:
            for _ in nxt:
                pass
        k.op("dve", lambda e: e.scalar_tensor_tensor(out=yacc[:], in0=x1p[par][:], scalar=float(ALPHA), in1=py[:], op0=ALU.mult, op1=ALU.add),
             reads=[x1p[par], py], writes=[yacc])
        _layer_norm(k, yacc, ot, lnt[2], lnt[3], st2, mv2, rstd2)
        k.dma("sp", xo[rows, :], ot[:], ot, reads=[ot], writes=[xo])
    k.barrier()
    es.close()
    k.release(mk0)


def build_peer():
    nc = bass.Bass("TRN2", target_bir_lowering=False)
    k = KB(nc)
    EI = "ExternalInput"
    T = {}
    T["x"] = k.dram("x", [TP, D], F32, EI)
    T["m0"] = k.dram("m0", [TP, D], F32, EI)
    T["m1"] = k.dram("m1", [TP, D], F32, EI)
    T["lnp"] = k.dram("lnp", [4, D], F32, EI)
    T["wq"] = k.dram("wq", [D, 2048], F32, EI)
    T["kk"] = k.dram("kk", [16, 128, 128], F32, EI)
    T["U"] = k.dram("U", [16384, D], F32, EI)
    T["V"] = k.dram("V", [16384, D], F32, EI)
    T["cstp"] = k.dram("cstp", [128, 256], F32, EI)
    T["xo"] = k.dram("xo", [TP, D], F32, "ExternalOutput")
    T["UV"] = k.dram("UV", [16384, 2 * D], BF16)
    peer_body(k, T, NTP)
    k.finish()
    return nc


def build_fused():
    nc = bass.Bass("TRN2", target_bir_lowering=False)
    k = KB(nc)
    EI = "ExternalInput"
    T = _mixer_drams(k, False, 2 * DEPTH)
    xin = k.dram("x", [S, D], F32, EI)
    out = k.dram("out", [S, D], F32, "ExternalOutput")
    xa = k.dram("xa", [S, D], F32)
    xb = k.dram("xb", [S, D], F32)
    mp = [k.dram("mp0", [S, D], F32), k.dram("mp1", [S, D], F32)]
    P = {}
    P["lnp"] = k.dram("lnp", [DEPTH, 4, D], F32, EI)
    P["wq"] = k.dram("wq", [DEPTH, D, 2048], F32, EI)
    P["kk"] = k.dram("kk", [DEPTH, 16, 128, 128], F32, EI)
    P["cstp"] = k.dram("cstp", [128, 256], F32, EI)
    UV = k.dram("UV", [16384, 2 * D], BF16)
    Us = [k.dram(f"U{l}", [16384, D], F32, EI) for l in range(DEPTH)]
    Vs = [k.dram(f"V{l}", [16384, D], F32, EI) for l in range(DEPTH)]
    src = xin
    for l in range(DEPTH):
        dst = out if l == DEPTH - 1 else (xa if l % 2 == 0 else xb)
        for c in range(2):
            Tv = _mixer_views(k, T, l * 2 + c)
            Tv["x"] = src
            Tv["m_part"] = mp[c]
            bg = (lambda es_, l=l: uv_convert_gen(k, Us[l], Vs[l], UV, es_, ("act", "dve"))) if c == 1 else None
            mixer_body(k, Tv, gates=("compute" if c == 0 else "reuse"), bg=bg)
        Pv = {"UV_ready": True, "x": src, "m0": mp[0], "m1": mp[1], "xo": dst, "cstp": P["cstp"], "U": Us[l], "V": Vs[l], "UV": UV}
        for n in ("lnp", "wq", "kk"):
            Pv[n] = k.view(P[n], P[n].ap[l])
        peer_body(k, Pv, S // 128)
        src = dst
    k.finish()
    return nc


def _consts_peer():
    c = np.zeros((128, 256), np.float32)
    c[:, 0:128] = np.eye(128, dtype=np.float32)
    c[:, 128:144] = np.arange(16, dtype=np.float32)[None, :]
    return c


def _peer_inputs(l, xh, m0h, m1h, inp, cstp):
    lnp = np.ascontiguousarray(np.stack([inp["ln1_g"][l], inp["ln1_b"][l], inp["ln2_g"][l], inp["ln2_b"][l]]).astype(np.float32))
    kk = np.ascontiguousarray(np.stack([inp["peer_k1"][l], inp["peer_k2"][l]], axis=1).reshape(16, 128, 128))
    return {"x": xh, "m0": m0h, "m1": m1h, "lnp": lnp, "wq": np.ascontiguousarray(inp["peer_wq"][l]), "kk": kk,
            "U": np.ascontiguousarray(inp["peer_u"][l]), "V": np.ascontiguousarray(inp["peer_v"][l]), "cstp": cstp}


_PROGS = {}


def kernel(**inputs):
    inp = {k_: np.asarray(v) for k_, v in inputs.items()}
    x = np.ascontiguousarray(inp["x"].astype(np.float32))
    if "fused" not in _PROGS:
        _PROGS["fused"] = build_fused()
    nc = _PROGS["fused"]
    consts = _consts_mixer()
    cstp = _consts_peer()
    shared = _fused_inputs(None, inp, consts, cstp)
    maps = []
    for core in range(8):
        d = dict(shared)
        d["x"] = np.ascontiguousarray(x[core // 2])
        maps.append(d)
    res = run_bass_kernel_spmd(nc, maps, core_ids=list(range(8)))
    return np.stack([res.results[2 * b]["out"] for b in range(4)]).astype(np.float32)
```

```python
from contextlib import ExitStack
import math
import os
import numpy as np
import ml_dtypes
import concourse.bass as bass
import concourse.mybir as mybir
from concourse.bass_utils import run_bass_kernel_spmd

F32 = mybir.dt.float32
BF16 = mybir.dt.bfloat16
I32 = mybir.dt.int32
U32 = mybir.dt.uint32
AF = mybir.ActivationFunctionType
ALU = mybir.AluOpType
AX = mybir.AxisListType

S = 4096
D = 1024
NT = S // 128
DEPTH = 4
ALPHA = (2 * DEPTH) ** 0.25
LN_EPS = 1e-5


class Buf:
    def __init__(self, name, ap):
        self.name = name
        self.ap = ap
        self.w = None
        self.r = {}
        self.dsem = None
        self.dcnt = 0

    def __getitem__(self, idx):
        return self.ap[idx]


class KB:
    CE = ("pe", "dve", "act", "pool")

    def __init__(self, nc):
        self.nc = nc
        self.e = dict(pe=nc.tensor, dve=nc.vector, act=nc.scalar, pool=nc.gpsimd, sp=nc.sync)
        self.sem = {k: nc.alloc_semaphore("c_" + k) for k in self.CE}
        self.bar = nc.alloc_semaphore("bar")
        self.nbar = 0
        self.cnt = {k: 0 for k in self.CE}
        self.seen = {}
        self.bufs = []
        self.uid = 0
        self.dsems = []
        self.sem_pool = []
        self.n_ops = 0

    def _nm(self, name):
        self.uid += 1
        return f"{name}_{self.uid}"

    def sb(self, name, shape, dt, es=None, side=None):
        nm = self._nm(name)
        kw = {} if side is None else {"side": side}
        if es is None:
            t = self.nc.alloc_sbuf_tensor(nm, list(shape), dt, **kw)
        else:
            t = es.enter_context(self.nc.sbuf_tensor(nm, list(shape), dt, **kw))
        b = Buf(nm, t.ap())
        self.bufs.append(b)
        return b

    def ps(self, name, shape, dt=F32, es=None):
        nm = self._nm(name)
        if es is None:
            t = self.nc.alloc_psum_tensor(nm, list(shape), dt)
        else:
            t = es.enter_context(self.nc.psum_tensor(nm, list(shape), dt))
        b = Buf(nm, t.ap())
        self.bufs.append(b)
        return b

    def dram(self, name, shape, dt, kind="Internal"):
        t = self.nc.dram_tensor(name, list(shape), dt, kind=kind)
        b = Buf(name, t.ap())
        self.bufs.append(b)
        return b

    def _wait(self, eng, tok):
        if tok[0] == "c":
            _, src, kk = tok
            if src == eng and eng == "pe":
                return
            key = (eng, "c", src)
            if self.seen.get(key, 0) >= kk:
                return
            self.seen[key] = kk
            self.e[eng].wait_ge(self.sem[src], kk)
        else:
            _, buf = tok
            key = (eng, "d", buf.name)
            kk = buf.dcnt
            if self.seen.get(key, 0) >= kk:
                return
            self.seen[key] = kk
            self.e[eng].wait_ge(buf.dsem, kk)

    def _deps(self, eng, reads, writes, nowaw=False):
        toks = []
        for b in reads:
            if b.w is not None:
                toks.append(b.w)
        for b in writes:
            if b.w is not None and not (nowaw and b.w[0] == "c" and b.w[1] == eng):
                toks.append(b.w)
            toks.extend(b.r.values())
        for t in toks:
            self._wait(eng, t)

    @staticmethod
    def _mark(tok, key, reads, writes):
        for b in reads:
            b.r[key] = tok
        for b in writes:
            b.w = tok
            b.r = {}

    def op(self, eng, fn, reads=(), writes=(), nowaw=False):
        self._deps(eng, reads, writes, nowaw)
        ins = fn(self.e[eng])
        self.cnt[eng] += 1
        ins.then_inc(self.sem[eng], 1)
        self._mark(("c", eng, self.cnt[eng]), eng, reads, writes)
        self.n_ops += 1
        return ins

    def dma(self, q, out_ap, in_ap, sbuf, reads=(), writes=(), indirect=None, **kw):
        self._deps(q, reads, writes)
        if sbuf.dsem is None:
            if self.sem_pool:
                sbuf.dsem, sbuf.dcnt = self.sem_pool.pop()
            else:
                sbuf.dsem = self.nc.alloc_semaphore("d_" + sbuf.name)
            self.dsems.append(sbuf)
        if indirect is None:
            ins = self.e[q].dma_start(out=out_ap, in_=in_ap, **kw)
        else:
            ins = self.e[q].indirect_dma_start(out=out_ap, out_offset=None, in_=in_ap, in_offset=indirect, **kw)
        sbuf.dcnt += 16
        ins.then_inc(sbuf.dsem, 16)
        self._mark(("d", sbuf), "d_" + sbuf.name, reads, writes)
        self.n_ops += 1
        return ins

    def coll(self, kind, op, groups, src, dst):
        self._deps("pool", [src], [dst])
        if dst.dsem is None:
            dst.dsem = self.nc.alloc_semaphore("d_" + dst.name)
            self.dsems.append(dst)
        ins = self.e["pool"].collective_compute(kind, op, replica_groups=groups, ins=[src.ap[:, :]], outs=[dst.ap[:, :]])
        dst.dcnt += 16
        ins.then_inc(dst.dsem, 16)
        self._mark(("d", dst), "d_" + dst.name, [src], [dst])
        return ins

    def barrier(self):
        sp = self.e["sp"]
        for e in self.CE:
            if self.cnt[e] > self.seen.get(("sp", "c", e), 0):
                self.seen[("sp", "c", e)] = self.cnt[e]
                sp.wait_ge(self.sem[e], self.cnt[e])
        for b in self.dsems:
            if b.dcnt > self.seen.get(("sp", "d", b.name), 0):
                self.seen[("sp", "d", b.name)] = b.dcnt
                sp.wait_ge(b.dsem, b.dcnt)
        self.nbar += 1
        sp.sem_inc(self.bar, 1)
        for e in self.CE:
            self.e[e].wait_ge(self.bar, self.nbar)
        for b in self.bufs:
            b.w = None
            b.r = {}

    def mark(self):
        return len(self.bufs)

    def release(self, marker):
        self.barrier()
        gone = self.bufs[marker:]
        del self.bufs[marker:]
        for b in gone:
            if b.dsem is not None:
                self.sem_pool.append((b.dsem, b.dcnt))
                self.dsems.remove(b)

    def view(self, buf, ap):
        return Buf(buf.name + "_v", ap)

    def finish(self):
        self.barrier()
        self.e["sp"].wait_ge(self.bar, self.nbar)


STG_N = 3072


class WLoader:
    def __init__(self, k, es, n=2, side=None):
        self.k = k
        self.stg = [k.sb("stg", [128, STG_N], F32, es, side=side) for _ in range(n)]
        self.i = 0

    def load(self, dst, dst_ap_fn, src_buf, src_rows0, kc, col0, ncols, q="sp"):
        k = self.k
        per = max(1, STG_N // ncols)
        c0 = 0
        while c0 < kc:
            c1 = min(kc, c0 + per)
            st = self.stg[self.i % len(self.stg)]
            self.i += 1
            src = src_buf.ap[src_rows0 + c0 * 128: src_rows0 + c1 * 128, col0:col0 + ncols].rearrange(
                "(c p) n -> p c n", p=128)
            sv = st.ap[:, 0:(c1 - c0) * ncols].rearrange("p (c n) -> p c n", n=ncols)
            k.dma(q, sv, src, st, reads=[src_buf], writes=[st])
            dv = dst_ap_fn(c0, c1)
            k.op("pool", lambda e: e.tensor_copy(out=dv, in_=sv), reads=[st], writes=[dst])
            c0 = c1


def mixer_body(k, T, gates="compute", bg=None):
    x, w_att, w_lru, w_ret, w_gate, w_br, w_out = (T[n] for n in ("x", "w_att", "w_lru", "w_ret", "w_gate", "w_br", "w_out"))
    lru_vec, wa_bd, wx_bd, tabA, tabR, dec, cst = (T[n] for n in ("lru_vec", "wa_bd", "wx_bd", "tabA", "tabR", "dec", "cst"))
    yaT, ybT, ycT, m_part = (T[n] for n in ("yaT", "ybT", "ycT", "m_part"))
    sgT = T["sgT"]
    mk0 = k.mark()
    eso = ExitStack()

    xT = k.sb("xT", [128, 8, S], BF16, eso)
    cs = k.sb("cs", [128, 512], F32, eso)
    identb = k.sb("identb", [128, 128], BF16, eso)
    onesb = k.sb("onesb", [128, 128], BF16, eso)
    decs = k.sb("decs", [128, 6], F32, eso)
    k.dma("sp", cs[:], cst[:], cs, reads=[cst], writes=[cs])
    k.dma("sp", decs[:], dec[:], decs, reads=[dec], writes=[decs])
    k.op("pool", lambda e: e.tensor_copy(out=identb[:], in_=cs[:, 0:128]), reads=[cs], writes=[identb])
    k.op("pool", lambda e: e.memset(onesb[:], 1.0), writes=[onesb])
    ident = cs

    with ExitStack() as es:
        xs = [k.sb("xs", [128, D], F32, es) for _ in range(2)]
        pT = [k.ps("pT", [128, 8, 128], F32, es) for _ in range(2)]
        for t in range(NT):
            xb = xs[t % 2]
            pb = pT[t % 2]
            k.dma("sp", xb[:], x[t * 128:(t + 1) * 128, :], xb, reads=[x], writes=[xb])
            for c in range(8):
                k.op("pe", lambda e: e.transpose(pb[:, c, :], xb[:, c * 128:(c + 1) * 128], ident[:, 0:128]),
                     reads=[xb, cs], writes=[pb])
            eng = "dve" if t % 2 == 0 else "act"
            if eng == "dve":
                k.op("dve", lambda e: e.tensor_copy(out=xT[:, :, t * 128:(t + 1) * 128], in_=pb[:]),
                     reads=[pb], writes=[xT])
            else:
                k.op("act", lambda e: e.activation(out=xT[:, :, t * 128:(t + 1) * 128], in_=pb[:], func=AF.Copy),
                     reads=[pb], writes=[xT])
        k.barrier()

    if gates == "compute":
        with ExitStack() as es:
            wl = WLoader(k, es)
            wgt = k.sb("wgt", [128, 8, 3072], BF16, es)
            for c0 in range(8):
                wl.load(wgt, lambda a, b, c0=c0: wgt[:, c0 + a:c0 + b, :], w_gate, c0 * 128, 1, 0, 3072)
            sgs = [k.sb("sgs", [128, 512], BF16, es) for _ in range(4)]
            pg_ = [k.ps("pgG", [128, 512], F32, es) for _ in range(4)]
            gi_ = 0
            for tg in range(8):
                tok = slice(tg * 512, (tg + 1) * 512)
                for m3 in range(24):
                    pb = pg_[gi_ % 4]
                    sb_ = sgs[gi_ % 4]
                    gi_ += 1
                    for c in range(8):
                        k.op("pe", lambda e: e.matmul(pb[:], lhsT=wgt[:, c, m3 * 128:(m3 + 1) * 128], rhs=xT[:, c, tok],
                                                      start=(c == 0), stop=(c == 7)), reads=[wgt, xT], writes=[pb])
                    k.op("act", lambda e: e.activation(out=sb_[:], in_=pb[:], func=AF.Sigmoid), reads=[pb], writes=[sb_])
                    k.dma("sp", sgT[m3 * 128:(m3 + 1) * 128, tok], sb_[:], sb_, reads=[sb_], writes=[sgT])
            k.barrier()

    if os.environ.get('MIX_STOP') == '0':
        eso.close()
        k.release(mk0)
        return
    TL = 1024
    with ExitStack() as es:
        wl = WLoader(k, es)
        wlru = k.sb("wlru", [128, 8, 1024], BF16, es)
        wl.load(wlru, lambda c0, c1: wlru[:, c0:c1, :], w_lru, 0, 8, 0, 1024)
        wabd = k.sb("wabd", [128, 4, 128], BF16, es)
        wxbd = k.sb("wxbd", [128, 4, 128], BF16, es)
        wl.load(wabd, lambda c0, c1: wabd[:, c0:c1, :], wa_bd, 0, 4, 0, 128)
        wl.load(wxbd, lambda c0, c1: wxbd[:, c0:c1, :], wx_bd, 0, 4, 0, 128)
        lv = k.sb("lv", [128, 4, 8], F32, es)
        k.dma("sp", lv[:], lru_vec.ap.rearrange("(c p) k -> p c k", p=128), lv, reads=[lru_vec], writes=[lv])
        ev = k.sb("ev", [128, 4], F32, es)
        zv = k.sb("zv", [128, 4], F32, es)
        z2 = k.sb("z2", [128, 4], F32, es)
        pv_ = k.sb("pv_", [128, 4], F32, es)
        nsp8 = k.sb("nsp8", [128, 4], F32, es)
        k.op("act", lambda e: e.activation(out=ev[:], in_=lv[:, :, 7], func=AF.Exp, scale=-1.0), reads=[lv], writes=[ev])
        k.op("dve", lambda e: e.tensor_scalar(out=zv[:], in0=ev[:], scalar1=2.0, scalar2=None, op0=ALU.add),
             reads=[ev], writes=[zv])
        k.op("dve", lambda e: e.reciprocal(out=zv[:], in_=zv[:]), reads=[zv], writes=[zv])
        k.op("dve", lambda e: e.tensor_tensor(out=zv[:], in0=zv[:], in1=ev[:], op=ALU.mult), reads=[zv, ev], writes=[zv])
        k.op("dve", lambda e: e.tensor_tensor(out=z2[:], in0=zv[:], in1=zv[:], op=ALU.mult), reads=[zv], writes=[z2])
        k.op("dve", lambda e: e.tensor_scalar(out=pv_[:], in0=z2[:], scalar1=1.0 / 7, scalar2=1.0 / 5, op0=ALU.mult, op1=ALU.add),
             reads=[z2], writes=[pv_])
        k.op("dve", lambda e: e.tensor_tensor(out=pv_[:], in0=pv_[:], in1=z2[:], op=ALU.mult), reads=[pv_, z2], writes=[pv_])
        k.op("dve", lambda e: e.tensor_scalar(out=pv_[:], in0=pv_[:], scalar1=1.0 / 3, scalar2=None, op0=ALU.add),
             reads=[pv_], writes=[pv_])
        k.op("dve", lambda e: e.tensor_tensor(out=pv_[:], in0=pv_[:], in1=z2[:], op=ALU.mult), reads=[pv_, z2], writes=[pv_])
        k.op("dve", lambda e: e.tensor_scalar(out=pv_[:], in0=pv_[:], scalar1=1.0, scalar2=None, op0=ALU.add),
             reads=[pv_], writes=[pv_])
        k.op("dve", lambda e: e.tensor_tensor(out=pv_[:], in0=pv_[:], in1=zv[:], op=ALU.mult), reads=[pv_, zv], writes=[pv_])
        k.op("dve", lambda e: e.tensor_scalar(out=nsp8[:], in0=pv_[:], scalar1=-16.0, scalar2=None, op0=ALU.mult),
             reads=[pv_], writes=[nsp8])

        lxs = [k.sb("lx", [128, TL + 3], F32, es) for _ in range(2)]
        glus = [k.sb("glu", [128, TL], F32, es) for _ in range(2)]
        xc = k.sb("xc", [128, TL], F32, es)
        xcb = k.sb("xcb", [128, TL], BF16, es)
        rr = k.sb("rr", [128, TL], F32, es)
        ii = k.sb("ii", [128, TL], F32, es)
        tt = k.sb("tt", [128, TL], F32, es)
        ybs = [k.sb("ybs", [128, TL], BF16, es) for _ in range(2)]
        hl = k.sb("hl", [128, 1], F32, es)
        pp = [k.ps("pl", [128, 512], F32, es) for _ in range(4)]
        pgt = [k.ps("plg", [128, 512], F32, es) for _ in range(2)]
        its = [(ch, tg) for ch in range(4) for tg in range(S // TL)]
        cnt = {"pp": 0, "pg": 0}

        def lru_f(i):
            ch, tg = its[i]
            t0 = tg * TL
            lx, glu = lxs[i % 2], glus[i % 2]
            if tg == 0:
                k.op("pool", lambda e: e.memset(lx[:, 0:3], 0.0), writes=[lx])
            else:
                lp = lxs[(i - 1) % 2]
                k.op("pool", lambda e: e.tensor_copy(out=lx[:, 0:3], in_=lp[:, TL:TL + 3]), reads=[lp], writes=[lx])
            for hf in range(TL // 512):
                for which in range(2):
                    pb = pp[cnt["pp"] % 4]
                    cnt["pp"] += 1
                    for c in range(8):
                        k.op("pe", lambda e: e.matmul(pb[:], lhsT=wlru[:, c, which * 512 + ch * 128: which * 512 + (ch + 1) * 128],
                                                      rhs=xT[:, c, t0 + hf * 512: t0 + (hf + 1) * 512],
                                                      start=(c == 0), stop=(c == 7)),
                             reads=[wlru, xT], writes=[pb])
                    if which == 0:
                        k.op("act", lambda e: e.activation(out=lx[:, 3 + hf * 512: 3 + (hf + 1) * 512], in_=pb[:], func=AF.Copy),
                             reads=[pb], writes=[lx])
                    else:
                        k.op("act", lambda e: e.activation(out=glu[:, hf * 512:(hf + 1) * 512], in_=pb[:], func=AF.Gelu),
                             reads=[pb], writes=[glu])

        def lru_g(i):
            ch, tg = its[i]
            t0 = tg * TL
            lx, glu, yb_ = lxs[i % 2], glus[i % 2], ybs[i % 2]
            k.op("dve", lambda e: e.tensor_scalar(out=xc[:], in0=lx[:, 3:3 + TL], scalar1=lv[:, ch, 3:4], scalar2=lv[:, ch, 4:5],
                                                  op0=ALU.mult, op1=ALU.add), reads=[lx, lv], writes=[xc])
            for j in range(3):
                k.op("dve", lambda e: e.scalar_tensor_tensor(out=xc[:], in0=lx[:, j:j + TL], scalar=lv[:, ch, j:j + 1], in1=xc[:],
                                                             op0=ALU.mult, op1=ALU.add), reads=[lx, lv, xc], writes=[xc])
            k.op("pool", lambda e: e.tensor_copy(out=xcb[:], in_=xc[:]), reads=[xc], writes=[xcb])
            for hf in range(TL // 512):
                for which in range(2):
                    pb = pgt[cnt["pg"] % 2]
                    cnt["pg"] += 1
                    wsel = wabd if which == 0 else wxbd
                    k.op("pe", lambda e: e.matmul(pb[:], lhsT=wsel[:, ch, :], rhs=xcb[:, hf * 512:(hf + 1) * 512], start=True, stop=True),
                         reads=[wsel, xcb], writes=[pb])
                    dst = rr if which == 0 else ii
                    bcol = 5 if which == 0 else 6
                    k.op("act", lambda e: e.activation(out=dst[:, hf * 512:(hf + 1) * 512], in_=pb[:], func=AF.Sigmoid,
                                                       bias=lv[:, ch, bcol:bcol + 1], scale=1.0), reads=[pb, lv], writes=[dst])
            k.op("act", lambda e: e.activation(out=rr[:], in_=rr[:], func=AF.Exp, scale=nsp8[:, ch:ch + 1]),
                 reads=[rr, nsp8], writes=[rr])
            k.op("dve", lambda e: e.tensor_tensor(out=tt[:], in0=rr[:], in1=rr[:], op=ALU.mult), reads=[rr], writes=[tt])
            k.op("act", lambda e: e.activation(out=tt[:], in_=tt[:], func=AF.Sqrt, scale=-1.0, bias=1.0), reads=[tt], writes=[tt])
            k.op("pool", lambda e: e.tensor_tensor(out=ii[:], in0=ii[:], in1=xc[:], op=ALU.mult), reads=[ii, xc], writes=[ii])
            k.op("dve", lambda e: e.tensor_tensor(out=tt[:], in0=tt[:], in1=ii[:], op=ALU.mult), reads=[tt, ii], writes=[tt])
            init = 0.0 if tg == 0 else hl[:, 0:1]
            k.op("dve", lambda e: e.tensor_tensor_scan(out=xc[:], data0=rr[:], data1=tt[:], initial=init, op0=ALU.mult, op1=ALU.add),
                 reads=[rr, tt, hl], writes=[xc])
            k.op("dve", lambda e: e.tensor_copy(out=hl[:], in_=xc[:, TL - 1:TL]), reads=[xc], writes=[hl])
            k.op("dve", lambda e: e.tensor_tensor(out=yb_[:], in0=xc[:], in1=glu[:], op=ALU.mult), reads=[xc, glu], writes=[yb_])
            k.dma("pool", ybT[ch * 128:(ch + 1) * 128, t0:t0 + TL], yb_[:], yb_, reads=[yb_], writes=[ybT])

        bgl = bg(es) if bg is not None else None
        lru_f(0)
        for i in range(len(its)):
            if i + 1 < len(its):
                lru_f(i + 1)
            for _ in range(4):
                if bgl is not None and next(bgl, "END") == "END":
                    bgl = None
            lru_g(i)
            for _ in range(4):
                if bgl is not None and next(bgl, "END") == "END":
                    bgl = None
        if bgl is not None:
            for _ in bgl:
                pass
        k.barrier()

    if os.environ.get('MIX_STOP') == '1':
        eso.close()
        k.release(mk0)
        return
    with ExitStack() as es:
        wl = WLoader(k, es)
        wret = k.sb("wret", [128, 8, 2048], BF16, es)
        wl.load(wret, lambda c0, c1: wret[:, c0:c1, :], w_ret, 0, 8, 0, 2048)
        tab = [k.sb("tabr", [128, 512], F32, es) for _ in range(2)]
        qs = k.sb("qs", [128, 2, 256], F32, es)
        ks = k.sb("ks", [128, 2, 256], F32, es)
        vb = k.sb("vb", [128, 2, 256], BF16, es)
        gs = k.sb("gs", [128, 2, 256], F32, es)
        t1 = k.sb("t1", [128, 2, 256], F32, es)
        t2 = k.sb("t2", [128, 2, 256], F32, es)
        qb = k.sb("qb", [128, 2, 256], BF16, es)
        kb_ = k.sb("kb", [128, 2, 256], BF16, es)
        qkT = k.sb("qkT", [128, 8, 128], BF16, es)
        PT = k.sb("PT", [128, 2, 128], BF16, es)
        W = k.sb("W", [128, 2, 2, 256], F32, es)
        Tb = k.sb("Tb", [128, 2, 2, 256], BF16, es)
        st = k.sb("st", [128, 2, 6], F32, es)
        mv = k.sb("mv", [128, 2, 2], F32, es)
        rstd = k.sb("rstd", [128, 2], F32, es)
        yn = k.sb("yn", [128, 2, 256], F32, es)
        ycb = k.sb("ycb", [128, 512], BF16, es)
        ycs = k.sb("ycs", [128, 4, 128], BF16, es)
        pq = k.ps("pq", [128, 512], F32, es)
        pk = k.ps("pk", [128, 512], F32, es)
        pv = k.ps("pv", [128, 512], F32, es)
        pg = k.ps("pg", [128, 512], F32, es)
        pTb = k.ps("pTb", [128, 8, 128], BF16, es)
        pS = k.ps("pS", [128, 2, 128], F32, es)
        pO = k.ps("pO", [128, 2, 256], F32, es)
        pD = k.ps("pD", [128, 2, 256], F32, es)
        k.op("pool", lambda e: e.memset(W[:], 0.0), writes=[W])
        k.op("pool", lambda e: e.memset(Tb[:], 0.0), writes=[Tb])
        maskR = cs[:, 128:256]
        qbs = [qb, k.sb("qb2", [128, 2, 256], BF16, es)]
        kbs = [kb_, k.sb("kb2", [128, 2, 256], BF16, es)]
        vbs2 = [vb, k.sb("vb2", [128, 2, 256], BF16, es)]
        gss = [gs, k.sb("gs2", [128, 2, 256], F32, es)]
        bgg = None

        def ret_f(t):
                tb_ = tab[t % 2]
                qb, kb_, vb, gs = qbs[t % 2], kbs[t % 2], vbs2[t % 2], gss[t % 2]
                k.dma("sp", tb_[:], tabR[t], tb_, reads=[tabR], writes=[tb_])
                tok = slice(t * 128, (t + 1) * 128)
                for pi, pb in enumerate((pq, pk, pv, pg)):
                    for c in range(8):
                        k.op("pe", lambda e: e.matmul(pb[:], lhsT=xT[:, c, tok], rhs=wret[:, c, pi * 512:(pi + 1) * 512],
                                                      start=(c == 0), stop=(c == 7)), reads=[xT, wret], writes=[pb])
                for h in range(2):
                    k.op("act", lambda e: e.activation(out=qs[:, h, :], in_=pq[:, h * 256:(h + 1) * 256], func=AF.Copy, scale=decs[:, h:h + 1]),
                         reads=[pq, decs], writes=[qs])
                    k.op("act", lambda e: e.activation(out=ks[:, h, :], in_=pk[:, h * 256:(h + 1) * 256], func=AF.Copy, scale=decs[:, 2 + h:3 + h]),
                         reads=[pk, decs], writes=[ks])
                k.op("act", lambda e: e.activation(out=vb[:].rearrange("p a b -> p (a b)"), in_=pv[:], func=AF.Copy), reads=[pv], writes=[vb])
                k.op("act", lambda e: e.activation(out=gs[:].rearrange("p a b -> p (a b)"), in_=pg[:], func=AF.Silu), reads=[pg], writes=[gs])
                CC = tb_[:, 0:256].rearrange("p (a b) -> p a b", a=2).unsqueeze(1).to_broadcast([128, 2, 2, 128])
                SS0 = tb_[:, 256:384].unsqueeze(1).to_broadcast([128, 2, 128])
                SS1 = tb_[:, 384:512].unsqueeze(1).to_broadcast([128, 2, 128])
                for src, dstb in ((qs, qb), (ks, kb_)):
                    X = src[:].rearrange("p h (a b) -> p h a b", a=2)
                    T1 = t1[:].rearrange("p h (a b) -> p h a b", a=2)
                    T2 = t2[:].rearrange("p h (a b) -> p h a b", a=2)
                    k.op("dve", lambda e: e.tensor_tensor(out=T1, in0=X, in1=CC, op=ALU.mult), reads=[src, tb_], writes=[t1])
                    k.op("pool", lambda e: e.tensor_tensor(out=T2[:, :, 0, :], in0=X[:, :, 1, :], in1=SS0, op=ALU.mult),
                         reads=[src, tb_], writes=[t2])
                    k.op("pool", lambda e: e.tensor_tensor(out=T2[:, :, 1, :], in0=X[:, :, 0, :], in1=SS1, op=ALU.mult),
                         reads=[src, tb_, t2], writes=[t2])
                    k.op("dve", lambda e: e.tensor_tensor(out=dstb[:], in0=t1[:], in1=t2[:], op=ALU.add), reads=[t1, t2], writes=[dstb])

        def ret_g(t):
                tok = slice(t * 128, (t + 1) * 128)
                qb, kb_, vb, gs = qbs[t % 2], kbs[t % 2], vbs2[t % 2], gss[t % 2]
                for h in range(2):
                    for kc in range(2):
                        k.op("pe", lambda e: e.transpose(pTb[:, h * 2 + kc, :], qb[:, h, kc * 128:(kc + 1) * 128], identb[:]),
                             reads=[qb, identb], writes=[pTb])
                        k.op("pe", lambda e: e.transpose(pTb[:, 4 + h * 2 + kc, :], kb_[:, h, kc * 128:(kc + 1) * 128], identb[:]),
                             reads=[kb_, identb], writes=[pTb])
                k.op("act", lambda e: e.activation(out=qkT[:], in_=pTb[:], func=AF.Copy), reads=[pTb], writes=[qkT])
                for h in range(2):
                    for kc in range(2):
                        k.op("pe", lambda e: e.matmul(pS[:, h, :], lhsT=qkT[:, 4 + h * 2 + kc, :], rhs=qkT[:, h * 2 + kc, :],
                                                      start=(kc == 0), stop=(kc == 1)), reads=[qkT], writes=[pS])
                    k.op("dve", lambda e: e.tensor_tensor(out=PT[:, h, :], in0=pS[:, h, :], in1=maskR, op=ALU.mult),
                         reads=[pS, cs], writes=[PT])
                    k.op("pe", lambda e: e.matmul(pO[:, h, :], lhsT=PT[:, h, :], rhs=vb[:, h, :], start=True, stop=False),
                         reads=[PT, vb], writes=[pO])
                    for kc in range(2):
                        k.op("pe", lambda e: e.matmul(pO[:, h, :], lhsT=qkT[:, h * 2 + kc, :], rhs=Tb[:, h, kc, :],
                                                      start=False, stop=(kc == 1)), reads=[qkT, Tb], writes=[pO])
                    for kc in range(2):
                        k.op("pe", lambda e: e.matmul(pD[:, kc, :], lhsT=kb_[:, h, kc * 128:(kc + 1) * 128], rhs=vb[:, h, :],
                                                      start=True, stop=True), reads=[kb_, vb], writes=[pD])
                    k.op("dve", lambda e: e.scalar_tensor_tensor(out=W[:, h].rearrange("p a b -> p (a b)"),
                                                                 in0=W[:, h].rearrange("p a b -> p (a b)"),
                                                                 scalar=decs[:, 4 + h:5 + h],
                                                                 in1=pD[:].rearrange("p a b -> p (a b)"), op0=ALU.mult, op1=ALU.add),
                         reads=[W, decs, pD], writes=[W])
                    k.op("act", lambda e: e.activation(out=Tb[:, h].rearrange("p a b -> p (a b)"), in_=W[:, h].rearrange("p a b -> p (a b)"),
                                                       func=AF.Copy, scale=decs[:, 4 + h:5 + h]), reads=[W, decs], writes=[Tb])
                    k.op("dve", lambda e: e.bn_stats(out=st[:, h, :], in_=pO[:, h, :]), reads=[pO], writes=[st])
                    k.op("dve", lambda e: e.bn_aggr(out=mv[:, h, :], in_=st[:, h, :]), reads=[st], writes=[mv])
                k.op("dve", lambda e: e.tensor_scalar(out=rstd[:], in0=mv[:, :, 1], scalar1=LN_EPS, scalar2=None, op0=ALU.add),
                     reads=[mv], writes=[rstd])
                k.op("act", lambda e: e.activation(out=rstd[:], in_=rstd[:], func=AF.Sqrt), reads=[rstd], writes=[rstd])
                k.op("dve", lambda e: e.reciprocal(out=rstd[:], in_=rstd[:]), reads=[rstd], writes=[rstd])
                for h in range(2):
                    k.op("dve", lambda e: e.tensor_scalar(out=yn[:, h, :], in0=pO[:, h, :], scalar1=mv[:, h, 0:1], scalar2=rstd[:, h:h + 1],
                                                          op0=ALU.subtract, op1=ALU.mult), reads=[pO, mv, rstd], writes=[yn])
                k.op("pool", lambda e: e.tensor_tensor(out=ycb[:], in0=yn[:].rearrange("p a b -> p (a b)"),
                                                       in1=gs[:].rearrange("p a b -> p (a b)"), op=ALU.mult), reads=[yn, gs], writes=[ycb])
                for c in range(4):
                    k.op("pe", lambda e: e.transpose(pTb[:, c, :], ycb[:, c * 128:(c + 1) * 128], identb[:]),
                         reads=[ycb, identb], writes=[pTb])
                k.op("act", lambda e: e.activation(out=ycs[:], in_=pTb[:, 0:4, :], func=AF.Copy), reads=[pTb], writes=[ycs])
                k.dma("sp", ycT.ap[:, tok].rearrange("(c p) t -> p c t", p=128), ycs[:], ycs, reads=[ycs], writes=[ycT])

        ret_f(0)
        for t in range(NT):
            for _ in range(4):
                if bgg is not None and next(bgg, "END") == "END":
                    bgg = None
            if t + 1 < NT:
                ret_f(t + 1)
            ret_g(t)
        if bgg is not None:
            for _ in bgg:
                pass
        k.barrier()

    if os.environ.get('MIX_STOP') == '2':
        eso.close()
        k.release(mk0)
        return
    with ExitStack() as es:
        wl = WLoader(k, es)
        watt = k.sb("watt", [128, 8, 768], BF16, es)
        acc = k.sb("acc", [128, 2, 2, S], F32, es)
        tba = [k.sb("tba", [128, 256], F32, es) for _ in range(2)]
        t1s = [k.sb("a_t1", [128, 4, 128], F32, es) for _ in range(2)]
        t2s = [k.sb("a_t2", [128, 4, 128], F32, es) for _ in range(2)]
        qkbs = [k.sb("qkb", [128, 4, 128], BF16, es) for _ in range(2)]
        vbs = [k.sb("avb", [128, 2, 128], BF16, es) for _ in range(3)]
        qkTs = [k.sb("aqkT", [128, 4, 128], BF16, es) for _ in range(3)]
        Ebs = [k.sb("Eb", [128, 2, 2, 128], BF16, es) for _ in range(2)]
        PTs = [k.sb("PTa", [128, 2, 2, 128], BF16, es) for _ in range(2)]
        pqks = [k.ps("pqk", [128, 512], F32, es) for _ in range(2)]
        pvas = [k.ps("pva", [128, 256], F32, es) for _ in range(2)]
        pTa = k.ps("pTa", [128, 4, 128], BF16, es)
        pSs = [k.ps("pSa", [128, 2, 2, 128], F32, es) for _ in range(2)]
        pN = k.ps("pND", [128, 2, 2, 128], F32, es)
        maskA = cs[:, 256:512].rearrange("p (a b) -> p a b", a=2)
        sc = 128.0 ** -0.5
        blocks = []
        for g, dil in enumerate((1, 4, 16)):
            for r in range(dil):
                for n in range(32 // dil):
                    blocks.append((g, dil, r, n, len(blocks)))

        def tok_of(dil, r, n):
            start = n * 128 * dil + r
            return slice(start, start + 127 * dil + 1, dil)

        def stage_f(blk):
            g, dil, r, n, bi = blk
            tok = tok_of(dil, r, n)
            tb_ = tba[bi % 2]
            t1, t2, qkb = t1s[bi % 2], t2s[bi % 2], qkbs[bi % 2]
            pqk, pva = pqks[bi % 2], pvas[bi % 2]
            vb_ = vbs[bi % 3]
            k.dma("sp", tb_[:], tabA[bi], tb_, reads=[tabA], writes=[tb_])
            for c in range(8):
                k.op("pe", lambda e: e.matmul(pqk[:], lhsT=xT[:, c, tok], rhs=watt[:, c, 0:512], start=(c == 0), stop=(c == 7)),
                     reads=[xT, watt], writes=[pqk])
            for c in range(8):
                k.op("pe", lambda e: e.matmul(pva[:], lhsT=xT[:, c, tok], rhs=watt[:, c, 512:768], start=(c == 0), stop=(c == 7)),
                     reads=[xT, watt], writes=[pva])
            X = pqk[:].rearrange("p (h a b) -> p h a b", h=4, a=2)
            T1 = t1[:].rearrange("p h (a b) -> p h a b", a=2)
            T2 = t2[:].rearrange("p h (a b) -> p h a b", a=2)
            CC = tb_[:, 0:128].rearrange("p (a b) -> p a b", a=2).unsqueeze(1).to_broadcast([128, 4, 2, 64])
            SS0 = tb_[:, 128:192].unsqueeze(1).to_broadcast([128, 4, 64])
            SS1 = tb_[:, 192:256].unsqueeze(1).to_broadcast([128, 4, 64])
            k.op("dve", lambda e: e.tensor_tensor(out=T1, in0=X, in1=CC, op=ALU.mult), reads=[pqk, tb_], writes=[t1])
            k.op("dve", lambda e: e.tensor_tensor(out=T2[:, :, 0, :], in0=X[:, :, 1, :], in1=SS0, op=ALU.mult),
                 reads=[pqk, tb_], writes=[t2])
            k.op("dve", lambda e: e.tensor_tensor(out=T2[:, :, 1, :], in0=X[:, :, 0, :], in1=SS1, op=ALU.mult),
                 reads=[pqk, tb_, t2], writes=[t2], nowaw=True)
            k.op("pool", lambda e: e.tensor_tensor(out=qkb[:], in0=t1[:], in1=t2[:], op=ALU.add), reads=[t1, t2], writes=[qkb])
            k.op("act", lambda e: e.activation(out=vb_[:].rearrange("p a b -> p (a b)"), in_=pva[:], func=AF.Copy),
                 reads=[pva], writes=[vb_])

        def stage_g(blk):
            g, dil, r, n, bi = blk
            tok = tok_of(dil, r, n)
            qkb = qkbs[bi % 2]
            qc, qp = qkTs[bi % 3], qkTs[(bi - 1) % 3]
            vc, vp = vbs[bi % 3], vbs[(bi - 1) % 3]
            pS, eb, pt = pSs[bi % 2], Ebs[bi % 2], PTs[bi % 2]
            lo = 0 if n > 0 else 1
            for j in range(4):
                k.op("pe", lambda e: e.transpose(pTa[:, j, :], qkb[:, j, :], identb[:]), reads=[qkb, identb], writes=[pTa])
            k.op("act", lambda e: e.activation(out=qc[:], in_=pTa[:], func=AF.Copy), reads=[pTa], writes=[qc])
            for hh in range(2):
                if n > 0:
                    k.op("pe", lambda e: e.matmul(pS[:, hh, 0, :], lhsT=qp[:, 2 + hh, :], rhs=qc[:, hh, :], start=True, stop=True),
                         reads=[qp, qc], writes=[pS])
                k.op("pe", lambda e: e.matmul(pS[:, hh, 1, :], lhsT=qc[:, 2 + hh, :], rhs=qc[:, hh, :], start=True, stop=True),
                     reads=[qc], writes=[pS])
            k.op("act", lambda e: e.activation(out=eb[:, :, lo:2, :], in_=pS[:, :, lo:2, :], func=AF.Exp, scale=sc),
                 reads=[pS], writes=[eb])
            mk_ = maskA[:, lo:2, :].unsqueeze(1).to_broadcast([128, 2, 2 - lo, 128])
            k.op("pool", lambda e: e.tensor_tensor(out=pt[:, :, lo:2, :], in0=eb[:, :, lo:2, :], in1=mk_, op=ALU.mult),
                 reads=[eb, cs], writes=[pt])
            for hh in range(2):
                for which in range(2):
                    lp = vp[:, hh, :] if which == 0 else onesb[:]
                    lc = vc[:, hh, :] if which == 0 else onesb[:]
                    if n > 0:
                        k.op("pe", lambda e: e.matmul(pN[:, hh, which, :], lhsT=lp, rhs=pt[:, hh, 0, :], start=True, stop=False),
                             reads=[vp, onesb, pt], writes=[pN])
                    k.op("pe", lambda e: e.matmul(pN[:, hh, which, :], lhsT=lc, rhs=pt[:, hh, 1, :], start=(n == 0), stop=True),
                         reads=[vc, onesb, pt], writes=[pN])
            av = acc[:, :, :, tok]
            if g == 0:
                k.op("dve", lambda e: e.tensor_copy(out=av, in_=pN[:]), reads=[pN], writes=[acc])
            else:
                k.op("dve", lambda e: e.tensor_tensor(out=av, in0=pN[:], in1=av, op=ALU.add), reads=[pN, acc], writes=[acc])

        def load_watt(g):
            wl.load(watt, lambda c0, c1: watt[:, c0:c1, :], w_att, 0, 8, g * 768, 768)

        load_watt(0)
        stage_f(blocks[0])
        for i, blk in enumerate(blocks):
            if i + 1 < len(blocks):
                nb_ = blocks[i + 1]
                if nb_[0] != blk[0]:
                    load_watt(nb_[0])
                stage_f(nb_)
            stage_g(blk)
        rd = k.sb("rd", [128, 1024], F32, es)
        yas = k.sb("yas", [128, 1024], BF16, es)
        for hh in range(2):
            for q4 in range(4):
                ts_ = slice(q4 * 1024, (q4 + 1) * 1024)
                k.op("dve", lambda e: e.reciprocal(out=rd[:], in_=acc[:, hh, 1, ts_]), reads=[acc], writes=[rd])
                k.op("dve", lambda e: e.tensor_tensor(out=yas[:], in0=acc[:, hh, 0, ts_], in1=rd[:], op=ALU.mult), reads=[acc, rd], writes=[yas])
                k.dma("sp", yaT[hh * 128:(hh + 1) * 128, ts_], yas[:], yas, reads=[yas], writes=[yaT])
        k.barrier()

    if os.environ.get('MIX_STOP') == '3':
        eso.close()
        k.release(mk0)
        return
    with ExitStack() as es:
        wl = WLoader(k, es, n=1)
        wbr = k.sb("wbr", [128, 10, 1024], BF16, es)
        wout = k.sb("wout", [128, 8, 1024], BF16, es)
        for c0 in range(0, 10, 2):
            wl.load(wbr, lambda a, b, c0=c0: wbr[:, c0 + a:c0 + b, :], w_br, c0 * 128, 2, 0, 1024)
        for c0 in range(0, 8, 2):
            wl.load(wout, lambda a, b, c0=c0: wout[:, c0 + a:c0 + b, :], w_out, c0 * 128, 2, 0, 1024)
        ya_s = k.sb("ya_s", [128, 2, 512], BF16, es)
        yb_s = k.sb("yb_s", [128, 4, 512], BF16, es)
        yc_s = k.sb("yc_s", [128, 4, 512], BF16, es)
        sgb = [k.sb("sgb", [128, 24, 512], BF16, es) for _ in range(2)]
        u1s = [k.sb("u1", [128, 512], F32, es) for _ in range(2)]
        u2s = [k.sb("u2", [128, 512], F32, es) for _ in range(2)]
        mT = k.sb("mT", [128, 8, 512], BF16, es)
        mo = [k.sb("mo", [128, 1024], F32, es) for _ in range(2)]
        PPs = [[k.ps("PP", [128, 512], F32, es) for _ in range(3)] for _ in range(2)]
        po = [k.ps("po", [128, 512], F32, es) for _ in range(2)]
        it = 0
        oi = 0
        for tg in range(8):
            tok = slice(tg * 512, (tg + 1) * 512)
            sg = sgb[tg % 2]
            k.dma("sp", sg[:], sgT.ap[:, tok].rearrange("(c p) t -> p c t", p=128), sg, reads=[sgT], writes=[sg])
            k.dma("sp", ya_s[:], yaT.ap[:, tok].rearrange("(c p) t -> p c t", p=128), ya_s, reads=[yaT], writes=[ya_s])
            k.dma("sp", yb_s[:], ybT.ap[:, tok].rearrange("(c p) t -> p c t", p=128), yb_s, reads=[ybT], writes=[yb_s])
            k.dma("sp", yc_s[:], ycT.ap[:, tok].rearrange("(c p) t -> p c t", p=128), yc_s, reads=[ycT], writes=[yc_s])
            for mc in range(8):
                PP = PPs[it % 2]
                u1, u2 = u1s[it % 2], u2s[it % 2]
                it += 1
                msl = slice(mc * 128, (mc + 1) * 128)
                for kc in range(2):
                    k.op("pe", lambda e: e.matmul(PP[0][:], lhsT=wbr[:, kc, msl], rhs=ya_s[:, kc, :], start=(kc == 0), stop=(kc == 1)),
                         reads=[wbr, ya_s], writes=[PP[0]])
                for kc in range(4):
                    k.op("pe", lambda e: e.matmul(PP[1][:], lhsT=wbr[:, 2 + kc, msl], rhs=yb_s[:, kc, :], start=(kc == 0), stop=(kc == 3)),
                         reads=[wbr, yb_s], writes=[PP[1]])
                for kc in range(4):
                    k.op("pe", lambda e: e.matmul(PP[2][:], lhsT=wbr[:, 6 + kc, msl], rhs=yc_s[:, kc, :], start=(kc == 0), stop=(kc == 3)),
                         reads=[wbr, yc_s], writes=[PP[2]])
                k.op("dve", lambda e: e.tensor_tensor(out=u1[:], in0=PP[0][:], in1=sg[:, mc, :], op=ALU.mult), reads=[PP[0], sg], writes=[u1])
                k.op("dve", lambda e: e.tensor_tensor(out=u2[:], in0=PP[1][:], in1=sg[:, 8 + mc, :], op=ALU.mult), reads=[PP[1], sg], writes=[u2])
                k.op("dve", lambda e: e.tensor_tensor(out=u1[:], in0=u1[:], in1=u2[:], op=ALU.add), reads=[u1, u2], writes=[u1])
                k.op("dve", lambda e: e.tensor_tensor(out=u2[:], in0=PP[2][:], in1=sg[:, 16 + mc, :], op=ALU.mult), reads=[PP[2], sg], writes=[u2])
                k.op("pool", lambda e: e.tensor_tensor(out=mT[:, mc, :], in0=u1[:], in1=u2[:], op=ALU.add), reads=[u1, u2], writes=[mT])
            for tt_ in range(4):
                mob = mo[oi % 2]
                oi += 1
                for half in range(2):
                    for mc in range(8):
                        k.op("pe", lambda e: e.matmul(po[half][:], lhsT=mT[:, mc, tt_ * 128:(tt_ + 1) * 128],
                                                      rhs=wout[:, mc, half * 512:(half + 1) * 512], start=(mc == 0), stop=(mc == 7)),
                             reads=[mT, wout], writes=[po[half]])
                    k.op("act", lambda e: e.activation(out=mob[:, half * 512:(half + 1) * 512], in_=po[half][:], func=AF.Copy),
                         reads=[po[half]], writes=[mob])
                r0 = tg * 512 + tt_ * 128
                k.dma("pool", m_part[r0:r0 + 128, :], mob[:], mob, reads=[mob], writes=[m_part])
        k.barrier()
    eso.close()
    k.release(mk0)


def _mixer_drams(k, debug=False, nl=1):
    EI = "ExternalInput"
    T = {}
    T["w_att"] = k.dram("w_att", [nl, D, 2304], F32, EI)
    T["w_lru"] = k.dram("w_lru", [nl, D, 1024], F32, EI)
    T["w_ret"] = k.dram("w_ret", [nl, D, 2048], F32, EI)
    T["w_gate"] = k.dram("w_gate", [nl, D, 3072], F32, EI)
    T["w_br"] = k.dram("w_br", [nl, 1280, D], F32, EI)
    T["w_out"] = k.dram("w_out", [nl, D, D], F32, EI)
    T["lru_vec"] = k.dram("lru_vec", [nl, 512, 8], F32, EI)
    T["wa_bd"] = k.dram("wa_bd", [nl, 512, 128], F32, EI)
    T["wx_bd"] = k.dram("wx_bd", [nl, 512, 128], F32, EI)
    T["dec"] = k.dram("dec", [nl, 128, 6], F32, EI)
    T["tabA"] = k.dram("tabA", [96, 128, 256], F32, EI)
    T["tabR"] = k.dram("tabR", [32, 128, 512], F32, EI)
    T["cst"] = k.dram("cst", [128, 512], F32, EI)
    kindS = "ExternalOutput" if debug else "Internal"
    T["yaT"] = k.dram("yaT", [256, S], BF16, kindS)
    T["ybT"] = k.dram("ybT", [512, S], BF16, kindS)
    T["ycT"] = k.dram("ycT", [512, S], BF16, kindS)
    T["sgT"] = k.dram("sgT", [3072, S], BF16)
    return T


PER_PASS = ("w_att", "w_lru", "w_ret", "w_gate", "w_br", "w_out", "lru_vec", "wa_bd", "wx_bd", "dec")


def _mixer_views(k, T, i):
    V = dict(T)
    for n in PER_PASS:
        V[n] = k.view(T[n], T[n].ap[i])
    return V


def build_mixer(debug=False):
    nc = bass.Bass("TRN2", target_bir_lowering=False)
    k = KB(nc)
    T = _mixer_drams(k, debug, 1)
    T["x"] = k.dram("x", [S, D], F32, "ExternalInput")
    T["m_part"] = k.dram("m_part", [S, D], F32, "ExternalOutput")
    mixer_body(k, _mixer_views(k, T, 0))
    k.finish()
    return nc


def _consts_mixer():
    i = np.arange(128)
    ident = np.eye(128, dtype=np.float32)
    maskR = (i[:, None] <= i[None, :]).astype(np.float32)
    maskP = (i[:, None] >= i[None, :]).astype(np.float32)
    maskC = (i[:, None] <= i[None, :]).astype(np.float32)
    cst = np.concatenate([ident, maskR, maskP, maskC], axis=1).astype(np.float32)
    pos = np.arange(S, dtype=np.float32)
    invA = (10000.0 ** (-np.arange(0, 128, 2, dtype=np.float32) / 128)).astype(np.float32)
    angA = pos[:, None] * invA[None, :]
    cA, sA = np.cos(angA).astype(np.float32), np.sin(angA).astype(np.float32)
    tabA = np.zeros((96, 128, 256), np.float32)
    bi = 0
    for dil in (1, 4, 16):
        nb = 32 // dil
        for r in range(dil):
            for n in range(nb):
                tk = (n * 128 + i) * dil + r
                tabA[bi] = np.concatenate([cA[tk], cA[tk], -sA[tk], sA[tk]], axis=1)
                bi += 1
    invR = (10000.0 ** (-np.arange(0, 256, 2, dtype=np.float32) / 256)).astype(np.float32)
    angR = pos[:, None] * invR[None, :]
    cR, sR = np.cos(angR).astype(np.float32), np.sin(angR).astype(np.float32)
    tabR = np.concatenate([cR, cR, -sR, sR], axis=1).reshape(32, 128, 512).astype(np.float32)
    return cst, tabA, tabR


def _dec(c):
    i = np.arange(128, dtype=np.float64)
    out = np.zeros((128, 6), np.float64)
    for hh in range(2):
        h = 2 * c + hh
        gam = 1.0 - 2.0 ** (-5.0 - h)
        out[:, hh] = gam ** i
        out[:, 2 + hh] = gam ** (-i) * 256 ** -0.5
        out[:, 4 + hh] = gam ** 128
    return out.astype(np.float32)


def _mixer_inputs(l, xb, c, inp, consts):
    cst, tabA, tabR = consts
    w_in = inp["w_in"][l]
    cols = []
    for g in range(3):
        for base in (0, 1536, 3072):
            for hh in range(2):
                hd = g * 4 + 2 * c + hh
                cols.append(np.arange(base + hd * 128, base + (hd + 1) * 128))
    w_att = np.ascontiguousarray(w_in[:, np.concatenate(cols)])
    w_lru = np.ascontiguousarray(np.concatenate([w_in[:, 4608 + c * 512: 4608 + (c + 1) * 512],
                                                 w_in[:, 5632 + c * 512: 5632 + (c + 1) * 512]], axis=1))
    w_ret = np.ascontiguousarray(np.concatenate([w_in[:, b0 + c * 512: b0 + (c + 1) * 512] for b0 in (6656, 7680, 8704, 9728)], axis=1))
    w_gate = np.ascontiguousarray(w_in[:, 10752:13824])
    wb = inp["w_branch"][l]
    w_br = np.ascontiguousarray(np.concatenate([wb[c * 256:(c + 1) * 256], wb[512 + c * 512: 512 + (c + 1) * 512],
                                                wb[1536 + c * 512: 1536 + (c + 1) * 512]], axis=0))
    sl = slice(c * 512, (c + 1) * 512)
    lru_vec = np.ascontiguousarray(np.stack([inp["conv_w"][l][0, sl], inp["conv_w"][l][1, sl], inp["conv_w"][l][2, sl],
                                             inp["conv_w"][l][3, sl], inp["conv_b"][l][sl], inp["lru_ba"][l][sl],
                                             inp["lru_bx"][l][sl], inp["lru_lambda"][l][sl]], axis=1).astype(np.float32))

    def bd(w):
        o = np.zeros((4, 128, 128), np.float32)
        for ch in range(4):
            for j in range(2):
                o[ch, j * 64:(j + 1) * 64, j * 64:(j + 1) * 64] = w[c * 8 + ch * 2 + j]
        return o.reshape(512, 128)

    d = {"w_att": w_att, "w_lru": w_lru, "w_ret": w_ret, "w_gate": w_gate, "w_br": w_br,
         "w_out": np.ascontiguousarray(inp["w_out"][l]), "lru_vec": lru_vec, "wa_bd": bd(inp["lru_wa"][l]),
         "wx_bd": bd(inp["lru_wx"][l]), "dec": _dec(c)}
    d = {k_: v[None] for k_, v in d.items()}
    if xb is not None:
        d.update({"x": xb, "tabA": tabA, "tabR": tabR, "cst": cst})
    return d


def _fused_inputs(xb, inp, consts, cstp):
    cst, tabA, tabR = consts
    per = [_mixer_inputs(l, None, c, inp, consts) for l in range(DEPTH) for c in range(2)]
    d = {n: np.ascontiguousarray(np.concatenate([p[n] for p in per], axis=0)) for n in PER_PASS}
    d.update({"x": xb, "tabA": tabA, "tabR": tabR, "cst": cst, "cstp": cstp})
    d["lnp"] = np.ascontiguousarray(np.stack([np.stack([inp["ln1_g"][l], inp["ln1_b"][l], inp["ln2_g"][l], inp["ln2_b"][l]])
                                               for l in range(DEPTH)]).astype(np.float32))
    d["wq"] = np.ascontiguousarray(inp["peer_wq"])
    d["kk"] = np.ascontiguousarray(np.stack([inp["peer_k1"], inp["peer_k2"]], axis=2).reshape(DEPTH, 16, 128, 128))
    for l in range(DEPTH):
        d[f"U{l}"] = np.ascontiguousarray(inp["peer_u"][l])
        d[f"V{l}"] = np.ascontiguousarray(inp["peer_v"][l])
    return d


TP = 2048
NTP = TP // 128
NG = 14


def _layer_norm(k, src, dst, gt, bt, st, mv, rstd):
    for j in range(2):
        k.op("dve", lambda e: e.bn_stats(out=st[:, j, :], in_=src[:, j * 512:(j + 1) * 512]), reads=[src], writes=[st])
    k.op("dve", lambda e: e.bn_aggr(out=mv[:], in_=st[:].rearrange("p a b -> p (a b)")), reads=[st], writes=[mv])
    k.op("dve", lambda e: e.tensor_scalar(out=rstd[:], in0=mv[:, 1:2], scalar1=LN_EPS, scalar2=None, op0=ALU.add),
         reads=[mv], writes=[rstd])
    k.op("act", lambda e: e.activation(out=rstd[:], in_=rstd[:], func=AF.Sqrt), reads=[rstd], writes=[rstd])
    k.op("dve", lambda e: e.reciprocal(out=rstd[:], in_=rstd[:]), reads=[rstd], writes=[rstd])
    k.op("dve", lambda e: e.tensor_scalar(out=dst[:], in0=src[:], scalar1=mv[:, 0:1], scalar2=rstd[:, 0:1],
                                          op0=ALU.subtract, op1=ALU.mult), reads=[src, mv, rstd], writes=[dst])
    k.op("dve", lambda e: e.tensor_tensor(out=dst[:], in0=dst[:], in1=gt[:], op=ALU.mult), reads=[dst, gt], writes=[dst])
    k.op("dve", lambda e: e.tensor_tensor(out=dst[:], in0=dst[:], in1=bt[:], op=ALU.add), reads=[dst, bt], writes=[dst])


def uv_convert_gen(k, U, V, UV, es0, engines):
    NB_ = 4
    cvs = [k.sb("cvs", [128, 2, D], F32, es0) for _ in range(NB_)]
    cvb = [k.sb("cvb", [128, 2, D], BF16, es0) for _ in range(NB_)]
    ci_ = 0
    for si_, src_ in enumerate((U, V)):
        for ch in range(64):
            a_, b_ = cvs[ci_ % NB_], cvb[ci_ % NB_]
            eng = engines[ci_ % len(engines)]
            ci_ += 1
            rs_ = slice(ch * 256, (ch + 1) * 256)
            k.dma("sp", a_[:], src_.ap[rs_, :].rearrange("(p j) d -> p j d", j=2), a_, reads=[src_], writes=[a_])
            if eng == "act":
                k.op("act", lambda e: e.activation(out=b_[:], in_=a_[:], func=AF.Copy), reads=[a_], writes=[b_])
            else:
                k.op(eng, lambda e: e.tensor_copy(out=b_[:], in_=a_[:]), reads=[a_], writes=[b_])
            k.dma("pool", UV.ap[rs_, si_ * D:(si_ + 1) * D].rearrange("(p j) d -> p j d", j=2), b_[:], b_, reads=[b_], writes=[UV])
            yield None


def peer_body(k, T, ntiles):
    x, m0, m1, lnp, wq, kk, U, V, cstp, xo = (T[n] for n in ("x", "m0", "m1", "lnp", "wq", "kk", "U", "V", "cstp", "xo"))
    mk0 = k.mark()

    UV = T["UV"]
    if not T.get("UV_ready"):
        with ExitStack() as es0:
            for _ in uv_convert_gen(k, U, V, UV, es0, ("act", "dve")):
                pass
            k.barrier()
    es = ExitStack()
    cs = k.sb("cs", [128, 256], F32, es)
    k.dma("sp", cs[:], cstp[:], cs, reads=[cstp], writes=[cs])
    ident = cs
    iota16 = cs[:, 128:144]
    lnt = [k.sb("lnt", [128, D], F32, es) for _ in range(4)]
    for j in range(4):
        k.dma("sp", lnt[j][:], lnp.ap[j].partition_broadcast(128), lnt[j], reads=[lnp], writes=[lnt[j]])
    es_s = ExitStack()
    wl = WLoader(k, es_s, side="right")
    wqb = k.sb("wqb", [128, 8, 2048], BF16, es)
    wl.load(wqb, lambda c0, c1: wqb[:, c0:c1, :], wq, 0, 8, 0, 2048)
    kks = k.sb("kks", [128, 16, 128], F32, es_s, side="right")
    kkT = k.sb("kkT", [128, 16, 128], BF16, es)
    k.dma("sp", kks[:], kk.ap.rearrange("h k c -> k h c"), kks, reads=[kk], writes=[kks])
    pT = k.ps("pT", [128, 8, 128], F32, es)
    for half in range(2):
        for j in range(8):
            k.op("pe", lambda e: e.transpose(pT[:, j, :], kks[:, half * 8 + j, :], ident[:, 0:128]), reads=[kks, cs], writes=[pT])
        k.op("dve", lambda e: e.tensor_copy(out=kkT[:, half * 8:(half + 1) * 8, :], in_=pT[:]), reads=[pT], writes=[kkT])

    xt = k.sb("xt", [128, D], F32, es)
    m0t = k.sb("m0t", [128, D], F32, es)
    m1t = k.sb("m1t", [128, D], F32, es)
    x1 = k.sb("x1", [128, D], F32, es)
    x1T = k.sb("x1T", [128, 8, 128], BF16, es)
    qT = k.sb("qT", [128, 16, 128], BF16, es)
    sc = k.sb("sc", [128, 16, 128], F32, es)
    sc2 = k.sb("sc2", [128, 16, 128], F32, es)
    v16 = k.sb("v16", [128, 16, 16], F32, es)
    i16 = k.sb("i16", [128, 16, 16], U32, es)
    i16f = k.sb("i16f", [128, 16, 16], F32, es)
    cand = k.sb("cand", [128, 8, 256], F32, es)
    cand2 = k.sb("cand2", [128, 8, 256], F32, es)
    cv = k.sb("cv", [128, 8, 16], F32, es)
    ci = k.sb("ci", [128, 8, 16], U32, es)
    hi_u = k.sb("hi_u", [128, 8, 16], U32, es)
    lo_u = k.sb("lo_u", [128, 8, 16], U32, es)
    hi_f = k.sb("hi_f", [128, 8, 16], F32, es)
    lo_f = k.sb("lo_f", [128, 8, 16], F32, es)
    eg = k.sb("eg", [128, 8, 16], F32, es)
    sm = k.sb("sm", [128, 8], F32, es)
    gg = k.sb("gg", [128, 128], F32, es)
    eq = k.sb("eq", [128, 8, 16, 16], F32, es)
    asel = k.sb("asel", [128, 8, 16], F32, es)
    bsel = k.sb("bsel", [128, 8, 16], F32, es)
    eidf = k.sb("eidf", [128, 128], F32, es)
    eid = k.sb("eid", [128, 128], U32, es)
    junk = k.sb("junk", [128, D], F32, es)
    yacc = k.sb("yacc", [128, D], F32, es)
    NR = 8
    pre_r = [k.sb("pre_r", [128, 1], F32, es) for _ in range(NR)]
    gl_r = [k.sb("gl_r", [128, 1], F32, es) for _ in range(NR)]
    dg_r = [k.sb("dg_r", [128, 128], BF16, es) for _ in range(NR)]
    identb = k.sb("identb", [128, 128], BF16, es)
    k.op("dve", lambda e: e.tensor_copy(out=identb[:], in_=cs[:, 0:128]), reads=[cs], writes=[identb])
    st = k.sb("st", [128, 2, 6], F32, es)
    mv = k.sb("mv", [128, 2], F32, es)
    rstd = k.sb("rstd", [128, 1], F32, es)
    k.barrier()
    es_s.close()
    gb = [k.sb("gb", [128, 2 * D], BF16, es) for _ in range(NG)]
    py = k.ps("py", [128, D], F32, es)
    pq = [k.ps("pq", [128, 4, 128], F32, es) for _ in range(2)]
    psc = [k.ps("psc", [128, 4, 128], F32, es) for _ in range(2)]
    x1p = [x1, k.sb("x1b", [128, D], F32, es)]
    ggp = [gg, k.sb("ggb", [128, 128], F32, es)]
    eidp = [eid, k.sb("eidb", [128, 128], U32, es)]
    ot = k.sb("ot", [128, D], F32, es)
    st2 = k.sb("st2", [128, 2, 6], F32, es)
    mv2 = k.sb("mv2", [128, 2], F32, es)
    rstd2 = k.sb("rstd2", [128, 1], F32, es)
    nt_run = int(os.environ.get("PEER_TILES", ntiles))

    def route_gen(t, par):
            rows = slice(t * 128, (t + 1) * 128)
            yield k.dma("sp", xt[:], x[rows, :], xt, reads=[x], writes=[xt])
            yield k.dma("sp", m0t[:], m0[rows, :], m0t, reads=[m0], writes=[m0t])
            yield k.dma("sp", m1t[:], m1[rows, :], m1t, reads=[m1], writes=[m1t])
            yield k.op("dve", lambda e: e.scalar_tensor_tensor(out=xt[:], in0=xt[:], scalar=float(ALPHA), in1=m0t[:], op0=ALU.mult, op1=ALU.add),
                 reads=[xt, m0t], writes=[xt])
            yield k.op("dve", lambda e: e.tensor_tensor(out=xt[:], in0=xt[:], in1=m1t[:], op=ALU.add), reads=[xt, m1t], writes=[xt])
            _layer_norm(k, xt, x1p[par], lnt[0], lnt[1], st, mv, rstd)
            yield None
            for c in range(8):
                yield k.op("pe", lambda e: e.transpose(pT[:, c, :], x1p[par][:, c * 128:(c + 1) * 128], ident[:, 0:128]), reads=[x1p[par], cs], writes=[pT])
            yield k.op("act", lambda e: e.activation(out=x1T[:], in_=pT[:], func=AF.Copy), reads=[pT], writes=[x1T])
            for q4 in range(4):
                pb = pq[q4 % 2]
                for j in range(4):
                    hc = q4 * 4 + j
                    for c in range(8):
                        yield k.op("pe", lambda e: e.matmul(pb[:, j, :], lhsT=wqb[:, c, hc * 128:(hc + 1) * 128], rhs=x1T[:, c, :],
                                                      start=(c == 0), stop=(c == 7)), reads=[wqb, x1T], writes=[pb])
                yield k.op("act", lambda e: e.activation(out=qT[:, q4 * 4:(q4 + 1) * 4, :], in_=pb[:], func=AF.Copy), reads=[pb], writes=[qT])
            for q4 in range(4):
                pb = psc[q4 % 2]
                for j in range(4):
                    hc = q4 * 4 + j
                    yield k.op("pe", lambda e: e.matmul(pb[:, j, :], lhsT=qT[:, hc, :], rhs=kkT[:, hc, :], start=True, stop=True),
                         reads=[qT, kkT], writes=[pb])
                yield k.op("act", lambda e: e.activation(out=sc[:, q4 * 4:(q4 + 1) * 4, :], in_=pb[:], func=AF.Copy), reads=[pb], writes=[sc])
            for hc in range(16):
                yield k.op("dve", lambda e: e.max(out=v16[:, hc, 0:8], in_=sc[:, hc, :]), reads=[sc], writes=[v16], nowaw=True)
            for hc in range(16):
                yield k.op("dve", lambda e: e.max_index(out=i16[:, hc, 0:8], in_max=v16[:, hc, 0:8], in_values=sc[:, hc, :]),
                     reads=[sc, v16], writes=[i16], nowaw=True)
            for hc in range(16):
                yield k.op("dve", lambda e: e.match_replace(out=sc2[:, hc, :], in_to_replace=v16[:, hc, 0:8], in_values=sc[:, hc, :], imm_value=-1e30),
                     reads=[sc, v16], writes=[sc2], nowaw=True)
            for hc in range(16):
                yield k.op("dve", lambda e: e.max(out=v16[:, hc, 8:16], in_=sc2[:, hc, :]), reads=[sc2], writes=[v16], nowaw=True)
            for hc in range(16):
                yield k.op("dve", lambda e: e.max_index(out=i16[:, hc, 8:16], in_max=v16[:, hc, 8:16], in_values=sc2[:, hc, :]),
                     reads=[sc2, v16], writes=[i16], nowaw=True)
            vv = v16[:].rearrange("p (h s) i -> p h s i", s=2)
            yield k.op("dve", lambda e: e.tensor_tensor(out=cand[:].rearrange("p h (i j) -> p h i j", i=16),
                                                  in0=vv[:, :, 0, :].unsqueeze(3).to_broadcast([128, 8, 16, 16]),
                                                  in1=vv[:, :, 1, :].unsqueeze(2).to_broadcast([128, 8, 16, 16]), op=ALU.add),
                 reads=[v16], writes=[cand])
            for h in range(8):
                yield k.op("dve", lambda e: e.max(out=cv[:, h, 0:8], in_=cand[:, h, :]), reads=[cand], writes=[cv], nowaw=True)
            for h in range(8):
                yield k.op("dve", lambda e: e.max_index(out=ci[:, h, 0:8], in_max=cv[:, h, 0:8], in_values=cand[:, h, :]),
                     reads=[cand, cv], writes=[ci], nowaw=True)
            for h in range(8):
                yield k.op("dve", lambda e: e.match_replace(out=cand2[:, h, :], in_to_replace=cv[:, h, 0:8], in_values=cand[:, h, :], imm_value=-1e30),
                     reads=[cand, cv], writes=[cand2], nowaw=True)
            for h in range(8):
                yield k.op("dve", lambda e: e.max(out=cv[:, h, 8:16], in_=cand2[:, h, :]), reads=[cand2], writes=[cv], nowaw=True)
            for h in range(8):
                yield k.op("dve", lambda e: e.max_index(out=ci[:, h, 8:16], in_max=cv[:, h, 8:16], in_values=cand2[:, h, :]),
                     reads=[cand2, cv], writes=[ci], nowaw=True)
            yield k.op("dve", lambda e: e.tensor_tensor(out=eg[:], in0=cv[:], in1=cv[:, :, 0:1].to_broadcast([128, 8, 16]), op=ALU.subtract),
                 reads=[cv], writes=[eg])
            yield k.op("act", lambda e: e.activation(out=eg[:], in_=eg[:], func=AF.Exp), reads=[eg], writes=[eg])
            yield k.op("dve", lambda e: e.tensor_reduce(out=sm[:], in_=eg[:], axis=AX.X, op=ALU.add), reads=[eg], writes=[sm])
            yield k.op("dve", lambda e: e.reciprocal(out=sm[:], in_=sm[:]), reads=[sm], writes=[sm])
            yield k.op("dve", lambda e: e.tensor_tensor(out=ggp[par][:].rearrange("p (h k) -> p h k", h=8), in0=eg[:],
                                                  in1=sm[:].unsqueeze(2).to_broadcast([128, 8, 16]), op=ALU.mult),
                 reads=[eg, sm], writes=[ggp[par]])
            yield k.op("dve", lambda e: e.tensor_single_scalar(out=hi_u[:], in_=ci[:], scalar=4, op=ALU.logical_shift_right), reads=[ci], writes=[hi_u])
            yield k.op("dve", lambda e: e.tensor_single_scalar(out=lo_u[:], in_=ci[:], scalar=15, op=ALU.bitwise_and), reads=[ci], writes=[lo_u])
            yield k.op("dve", lambda e: e.tensor_copy(out=hi_f[:], in_=hi_u[:]), reads=[hi_u], writes=[hi_f])
            yield k.op("dve", lambda e: e.tensor_copy(out=lo_f[:], in_=lo_u[:]), reads=[lo_u], writes=[lo_f])
            yield k.op("dve", lambda e: e.tensor_copy(out=i16f[:], in_=i16[:]), reads=[i16], writes=[i16f])
            iv = i16f[:].rearrange("p (h s) i -> p h s i", s=2)
            io = iota16.unsqueeze(1).unsqueeze(1).to_broadcast([128, 8, 16, 16])
            for sel, src, dsts in ((0, hi_f, asel), (1, lo_f, bsel)):
                yield k.op("dve", lambda e: e.tensor_tensor(out=eq[:], in0=src[:].unsqueeze(3).to_broadcast([128, 8, 16, 16]), in1=io, op=ALU.is_equal),
                     reads=[src, cs], writes=[eq])
                yield k.op("dve", lambda e: e.tensor_tensor(out=eq[:], in0=eq[:], in1=iv[:, :, sel, :].unsqueeze(2).to_broadcast([128, 8, 16, 16]),
                                                      op=ALU.mult), reads=[eq, i16f], writes=[eq])
                yield k.op("dve", lambda e: e.tensor_reduce(out=dsts[:], in_=eq[:], axis=AX.X, op=ALU.add), reads=[eq], writes=[dsts])
            yield k.op("dve", lambda e: e.scalar_tensor_tensor(out=eidf[:], in0=asel[:].rearrange("p h k -> p (h k)"), scalar=128.0,
                                                         in1=bsel[:].rearrange("p h k -> p (h k)"), op0=ALU.mult, op1=ALU.add),
                 reads=[asel, bsel], writes=[eidf])
            yield k.op("dve", lambda e: e.tensor_copy(out=eidp[par][:], in_=eidf[:]), reads=[eidf], writes=[eidp[par]])

    gi = 0
    for _ in route_gen(0, 0):
        pass
    for t in range(nt_run):
        par = t % 2
        rows = slice(t * 128, (t + 1) * 128)
        nxt = route_gen(t + 1, 1 - par) if t + 1 < nt_run else None
        for hk in range(128):
            g_ = gb[gi % NG]
            pr_, gl_, dg_ = pre_r[gi % NR], gl_r[gi % NR], dg_r[gi % NR]
            gi += 1
            k.dma("pool", g_[:], UV[:, :], g_, reads=[UV, eidp[par]], writes=[g_],
                  indirect=bass.IndirectOffsetOnAxis(ap=eidp[par][:, hk:hk + 1], axis=0))
            k.op("dve", lambda e: e.scalar_tensor_tensor(out=junk[:], in0=g_[:, 0:D], scalar=1.0, in1=x1p[par][:],
                                                         op0=ALU.mult, op1=ALU.mult, accum_out=pr_[:]),
                 reads=[g_, x1p[par]], writes=[pr_])
            k.op("act", lambda e: e.activation(out=gl_[:], in_=pr_[:], func=AF.Gelu), reads=[pr_], writes=[gl_])
            k.op("act", lambda e: e.activation(out=gl_[:], in_=gl_[:], func=AF.Copy, scale=ggp[par][:, hk:hk + 1]),
                 reads=[gl_, ggp[par]], writes=[gl_])
            k.op("act", lambda e: e.activation(out=dg_[:], in_=identb[:], func=AF.Copy, scale=gl_[:, 0:1]),
                 reads=[identb, gl_], writes=[dg_])
            for half in range(2):
                k.op("pe", lambda e: e.matmul(py[:, half * 512:(half + 1) * 512], lhsT=dg_[:], rhs=g_[:, D + half * 512: D + (half + 1) * 512],
                                              start=(hk == 0), stop=(hk == 127)), reads=[dg_, g_], writes=[py])
            for _ in range(3):
                if nxt is not None and next(nxt, "END") == "END":
                    nxt = None
        if nxt is not None:
            for _ in nxt:
                pass
        k.op("dve", lambda e: e.scalar_tensor_tensor(out=yacc[:], in0=x1p[par][:], scalar=float(ALPHA), in1=py[:], op0=ALU.mult, op1=ALU.add),
             reads=[x1p[par], py], writes=[yacc])
        _layer_norm(k, yacc, ot, lnt[2], lnt[3], st2, mv2, rstd2)
        k.dma("sp", xo[rows, :], ot[:], ot, reads=[ot], writes=[xo])
    k.barrier()
    es.close()
    k.release(mk0)


def build_peer():
    nc = bass.Bass("TRN2", target_bir_lowering=False)
    k = KB(nc)
    EI = "ExternalInput"
    T = {}
    T["x"] = k.dram("x", [TP, D], F32, EI)
    T["m0"] = k.dram("m0", [TP, D], F32, EI)
    T["m1"] = k.dram("m1", [TP, D], F32, EI)
    T["lnp"] = k.dram("lnp", [4, D], F32, EI)
    T["wq"] = k.dram("wq", [D, 2048], F32, EI)
    T["kk"] = k.dram("kk", [16, 128, 128], F32, EI)
    T["U"] = k.dram("U", [16384, D], F32, EI)
    T["V"] = k.dram("V", [16384, D], F32, EI)
    T["cstp"] = k.dram("cstp", [128, 256], F32, EI)
    T["xo"] = k.dram("xo", [TP, D], F32, "ExternalOutput")
    T["UV"] = k.dram("UV", [16384, 2 * D], BF16)
    peer_body(k, T, NTP)
    k.finish()
    return nc


def build_fused():
    nc = bass.Bass("TRN2", target_bir_lowering=False)
    k = KB(nc)
    EI = "ExternalInput"
    T = _mixer_drams(k, False, 2 * DEPTH)
    xin = k.dram("x", [S, D], F32, EI)
    out = k.dram("out", [S, D], F32, "ExternalOutput")
    xa = k.dram("xa", [S, D], F32)
    xb = k.dram("xb", [S, D], F32)
    mp = [k.dram("mp0", [S, D], F32), k.dram("mp1", [S, D], F32)]
    P = {}
    P["lnp"] = k.dram("lnp", [DEPTH, 4, D], F32, EI)
    P["wq"] = k.dram("wq", [DEPTH, D, 2048], F32, EI)
    P["kk"] = k.dram("kk", [DEPTH, 16, 128, 128], F32, EI)
    P["cstp"] = k.dram("cstp", [128, 256], F32, EI)
    UV = k.dram("UV", [16384, 2 * D], BF16)
    Us = [k.dram(f"U{l}", [16384, D], F32, EI) for l in range(DEPTH)]
    Vs = [k.dram(f"V{l}", [16384, D], F32, EI) for l in range(DEPTH)]
    src = xin
    for l in range(DEPTH):
        dst = out if l == DEPTH - 1 else (xa if l % 2 == 0 else xb)
        for c in range(2):
            Tv = _mixer_views(k, T, l * 2 + c)
            Tv["x"] = src
            Tv["m_part"] = mp[c]
            bg = (lambda es_, l=l: uv_convert_gen(k, Us[l], Vs[l], UV, es_, ("act", "dve"))) if c == 1 else None
            mixer_body(k, Tv, gates=("compute" if c == 0 else "reuse"), bg=bg)
        Pv = {"UV_ready": True, "x": src, "m0": mp[0], "m1": mp[1], "xo": dst, "cstp": P["cstp"], "U": Us[l], "V": Vs[l], "UV": UV}
        for n in ("lnp", "wq", "kk"):
            Pv[n] = k.view(P[n], P[n].ap[l])
        peer_body(k, Pv, S // 128)
        src = dst
    k.finish()
    return nc


def _consts_peer():
    c = np.zeros((128, 256), np.float32)
    c[:, 0:128] = np.eye(128, dtype=np.float32)
    c[:, 128:144] = np.arange(16, dtype=np.float32)[None, :]
    return c


def _peer_inputs(l, xh, m0h, m1h, inp, cstp):
    lnp = np.ascontiguousarray(np.stack([inp["ln1_g"][l], inp["ln1_b"][l], inp["ln2_g"][l], inp["ln2_b"][l]]).astype(np.float32))
    kk = np.ascontiguousarray(np.stack([inp["peer_k1"][l], inp["peer_k2"][l]], axis=1).reshape(16, 128, 128))
    return {"x": xh, "m0": m0h, "m1": m1h, "lnp": lnp, "wq": np.ascontiguousarray(inp["peer_wq"][l]), "kk": kk,
            "U": np.ascontiguousarray(inp["peer_u"][l]), "V": np.ascontiguousarray(inp["peer_v"][l]), "cstp": cstp}


_PROGS = {}


def kernel(**inputs):
    inp = {k_: np.asarray(v) for k_, v in inputs.items()}
    x = np.ascontiguousarray(inp["x"].astype(np.float32))
    if "fused" not in _PROGS:
        _PROGS["fused"] = build_fused()
    nc = _PROGS["fused"]
    consts = _consts_mixer()
    cstp = _consts_peer()
    shared = _fused_inputs(None, inp, consts, cstp)
    maps = []
    for core in range(8):
        d = dict(shared)
        d["x"] = np.ascontiguousarray(x[core // 2])
        maps.append(d)
    res = run_bass_kernel_spmd(nc, maps, core_ids=list(range(8)))
    return np.stack([res.results[2 * b]["out"] for b in range(4)]).astype(np.float32)
```
